# Optimizing a Trainium2 kernel written in Bass

```python
import math
import jax, jax.numpy as jnp
from jax import lax
import numpy as np

D_MODEL = 1024
BATCH = 4
SEQ = 8192
DEPTH = 1

HEAD_DIM = 64
N_HEADS = D_MODEL // HEAD_DIM
D_MIX = N_HEADS * HEAD_DIM
DIL_HEADS = 6
NSA_HEADS = N_HEADS - DIL_HEADS
DIL_PATTERNS = ((128, 1), (512, 4), (2048, 16))
NSA_KV_HEADS = 2
NSA_GROUP = NSA_HEADS // NSA_KV_HEADS
CMP_BLOCK = 32
CMP_STRIDE = 16
CMP_HIDDEN = 256
SEL_BLOCK = 64
SEL_TOPK = 16
WIN = 512
FORCE_SCORE = 1.0e4
N_BUCKETS = 32
MAX_DISTANCE = 2048
N_GROUPS = 4
EXPERTS_PER_GROUP = 4
N_EXPERTS = N_GROUPS * EXPERTS_PER_GROUP
D_EXPERT = 512
TOPK_IN_GROUP = 2
Q_BLOCK = 128
EPS = 1e-6
N_IN = 3 * DIL_HEADS * HEAD_DIM + NSA_HEADS * HEAD_DIM + 6 * NSA_KV_HEADS * HEAD_DIM + 3 * NSA_HEADS

kernel_name = "hybrid_dilated_nsa_hiermoe"


def rmsnorm(x, g):
    xf = x.astype(jnp.float32)
    y = xf * lax.rsqrt(jnp.mean(xf * xf, axis=-1, keepdims=True) + EPS)
    return (y * g.astype(jnp.float32)).astype(x.dtype)


def t5_bucket(dist):
    dist = jnp.maximum(dist, 0)
    max_exact = N_BUCKETS // 2
    large = max_exact + (jnp.log(jnp.maximum(dist, 1).astype(jnp.float32) / max_exact)
                         / math.log(MAX_DISTANCE / max_exact) * (N_BUCKETS - max_exact)).astype(jnp.int32)
    large = jnp.minimum(large, N_BUCKETS - 1)
    return jnp.where(dist < max_exact, dist, large)


def masked_softmax(s, mask, axis):
    s = jnp.where(mask, s, -jnp.inf)
    m = jnp.max(s, axis=axis, keepdims=True)
    m = jnp.where(jnp.isfinite(m), m, 0.0)
    e = jnp.where(mask, jnp.exp(s - m), 0.0)
    den = jnp.sum(e, axis=axis, keepdims=True)
    p = e / jnp.maximum(den, 1e-30)
    return p, m + jnp.log(den)


def compress(tok, pos_emb, w1, w2, n_cmp):
    B = tok.shape[0]
    idx = jnp.arange(n_cmp)[:, None] * CMP_STRIDE + jnp.arange(CMP_BLOCK)[None, :]
    blocks = jnp.take(tok, idx, axis=1) + pos_emb[None, None, :, None, :]
    flat = blocks.transpose(0, 1, 3, 2, 4).reshape(B, n_cmp, NSA_KV_HEADS, CMP_BLOCK * HEAD_DIM)
    return jax.nn.gelu(flat @ w1) @ w2


def hybrid_mixer(xn, w_in, w_out, cmp_pos_k, cmp_pos_v, cmp_k_w1, cmp_k_w2, cmp_v_w1, cmp_v_w2, rel_bias):
    B, T, _ = xn.shape
    scale = 1.0 / math.sqrt(HEAD_DIM)
    proj = xn @ w_in
    sizes = [DIL_HEADS * HEAD_DIM] * 3 + [NSA_HEADS * HEAD_DIM] + [NSA_KV_HEADS * HEAD_DIM] * 6 + [3 * NSA_HEADS]
    parts = jnp.split(proj, np.cumsum(sizes)[:-1].tolist(), axis=-1)
    q_a, k_a, v_a = [p.reshape(B, T, DIL_HEADS, HEAD_DIM) for p in parts[:3]]
    q_b = parts[3].reshape(B, T, NSA_KV_HEADS, NSA_GROUP, HEAD_DIM)
    k_ct, v_ct, k_s, v_s, k_w, v_w = [p.reshape(B, T, NSA_KV_HEADS, HEAD_DIM) for p in parts[4:10]]
    gates = jax.nn.sigmoid(parts[10].astype(jnp.float32)).reshape(B, T, NSA_KV_HEADS, NSA_GROUP, 3)

    bias_a = rel_bias[:, :DIL_HEADS].T
    bias_b = rel_bias[:, DIL_HEADS:].T.reshape(NSA_KV_HEADS, NSA_GROUP, N_BUCKETS)

    n_cmp = (T - CMP_BLOCK) // CMP_STRIDE + 1
    k_cmp = compress(k_ct, cmp_pos_k, cmp_k_w1, cmp_k_w2, n_cmp)
    v_cmp = compress(v_ct, cmp_pos_v, cmp_v_w1, cmp_v_w2, n_cmp)
    cmp_start = jnp.arange(n_cmp) * CMP_STRIDE
    cmp_end = cmp_start + CMP_BLOCK - 1
    n_sel = T // SEL_BLOCK
    n_top = min(SEL_TOPK, n_sel)
    sel_start = jnp.arange(n_sel) * SEL_BLOCK
    overlap = jnp.clip(jnp.minimum(cmp_start[:, None] + CMP_BLOCK, sel_start[None, :] + SEL_BLOCK)
                       - jnp.maximum(cmp_start[:, None], sel_start[None, :]), 0).astype(jnp.float32) / CMP_BLOCK
    k_sel_blocks = k_s.reshape(B, n_sel, SEL_BLOCK, NSA_KV_HEADS, HEAD_DIM).transpose(0, 3, 1, 2, 4)
    v_sel_blocks = v_s.reshape(B, n_sel, SEL_BLOCK, NSA_KV_HEADS, HEAD_DIM).transpose(0, 3, 1, 2, 4)
    pad = ((0, 0), (WIN, 0), (0, 0), (0, 0))
    k_w_pad = jnp.pad(k_w, pad)
    v_w_pad = jnp.pad(v_w, pad)

    bi = jnp.arange(B)[:, None, None]
    hi = jnp.arange(NSA_KV_HEADS)[None, :, None]
    hi6 = jnp.arange(NSA_KV_HEADS)[None, :, None, None, None, None]
    gi6 = jnp.arange(NSA_GROUP)[None, None, :, None, None, None]

    def block_fn(b):
        t0 = b * Q_BLOCK
        t = t0 + jnp.arange(Q_BLOCK)
        qa = lax.dynamic_slice_in_dim(q_a, t0, Q_BLOCK, axis=1)
        outs, lses = [], []
        for (w, d) in DIL_PATTERNS:
            dist = d * jnp.arange(w // d + 1)
            idx = t[:, None] - dist[None, :]
            valid = idx >= 0
            idx = jnp.maximum(idx, 0)
            kg = jnp.take(k_a, idx, axis=1)
            vg = jnp.take(v_a, idx, axis=1)
            s = jnp.einsum('bqhd,bqkhd->bhqk', qa, kg, preferred_element_type=jnp.float32) * scale
            s = s + bias_a[:, t5_bucket(dist)][None, :, None, :].astype(jnp.float32)
            p, lse = masked_softmax(s, valid[None, None], -1)
            outs.append(jnp.einsum('bhqk,bqkhd->bqhd', p, vg))
            lses.append(lse)
        wts = jax.nn.softmax(jnp.stack(lses, axis=0), axis=0)
        o_a = sum(jnp.swapaxes(wts[i], 1, 2) * outs[i] for i in range(len(DIL_PATTERNS)))

        qb = lax.dynamic_slice_in_dim(q_b, t0, Q_BLOCK, axis=1)
        dist_c = t[:, None] - cmp_end[None, :]
        s_c = jnp.einsum('bqkgd,bnkd->bkgqn', qb, k_cmp, preferred_element_type=jnp.float32) * scale
        s_c = s_c + bias_b[:, :, t5_bucket(dist_c)][None].astype(jnp.float32)
        p_c, _ = masked_softmax(s_c, (dist_c >= 0)[None, None, None], -1)
        o_c = jnp.einsum('bkgqn,bnkd->bqkgd', p_c, v_cmp)
        imp = jnp.einsum('bkgqn,ns->bkqs', p_c, overlap)
        blk = jnp.arange(n_sel)[None, :]
        cur = (t // SEL_BLOCK)[:, None]
        forced = (blk == cur) | (blk == cur - 1) | (blk == 0)
        imp = jnp.where(forced, FORCE_SCORE, jnp.where(blk <= cur, imp, -1.0))
        _, sel = lax.top_k(imp, n_top)
        sel_flat = sel.reshape(B, NSA_KV_HEADS, Q_BLOCK * n_top)
        kg = k_sel_blocks[bi, hi, sel_flat].reshape(B, NSA_KV_HEADS, Q_BLOCK, n_top, SEL_BLOCK, HEAD_DIM)
        vg = v_sel_blocks[bi, hi, sel_flat].reshape(B, NSA_KV_HEADS, Q_BLOCK, n_top, SEL_BLOCK, HEAD_DIM)
        pos = sel[..., None] * SEL_BLOCK + jnp.arange(SEL_BLOCK)
        dist_s = t[None, None, :, None, None] - pos
        s_s = jnp.einsum('bqkgd,bkqnld->bkgqnl', qb, kg, preferred_element_type=jnp.float32) * scale
        s_s = s_s + bias_b[hi6, gi6, t5_bucket(dist_s)[:, :, None]].astype(jnp.float32)
        flat_shape = (B, NSA_KV_HEADS, NSA_GROUP, Q_BLOCK, n_top * SEL_BLOCK)
        mask_s = jnp.broadcast_to((dist_s >= 0)[:, :, None], s_s.shape).reshape(flat_shape)
        p_s, _ = masked_softmax(s_s.reshape(flat_shape), mask_s, -1)
        p_s = p_s.reshape(s_s.shape)
        o_s = jnp.einsum('bkgqnl,bkqnld->bqkgd', p_s, vg)
        kw = lax.dynamic_slice_in_dim(k_w_pad, t0, Q_BLOCK + WIN, axis=1)
        vw = lax.dynamic_slice_in_dim(v_w_pad, t0, Q_BLOCK + WIN, axis=1)
        s_pos = t0 - WIN + jnp.arange(Q_BLOCK + WIN)
        dist_w = t[:, None] - s_pos[None, :]
        mask_w = (dist_w >= 0) & (dist_w < WIN) & (s_pos[None, :] >= 0)
        s_w = jnp.einsum('bqkgd,bskd->bkgqs', qb, kw, preferred_element_type=jnp.float32) * scale
        s_w = s_w + bias_b[:, :, t5_bucket(dist_w)][None].astype(jnp.float32)
        p_w, _ = masked_softmax(s_w, mask_w[None, None, None], -1)
        o_w = jnp.einsum('bkgqs,bskd->bqkgd', p_w, vw)
        g = lax.dynamic_slice_in_dim(gates, t0, Q_BLOCK, axis=1)
        o_b = g[..., 0:1] * o_c + g[..., 1:2] * o_s + g[..., 2:3] * o_w
        return jnp.concatenate([o_a.reshape(B, Q_BLOCK, DIL_HEADS * HEAD_DIM),
                                o_b.reshape(B, Q_BLOCK, NSA_HEADS * HEAD_DIM)], axis=-1)

    ys = lax.map(block_fn, jnp.arange(T // Q_BLOCK))
    y = ys.transpose(1, 0, 2, 3).reshape(B, T, D_MIX).astype(xn.dtype)
    return (y @ w_out).astype(xn.dtype)


def hier_moe(h, w_router_group, b_router_group, w_router_expert, b_router_expert, w_gate, w_up, w_down):
    B, T, D = h.shape
    xf = h.reshape(B * T, D)
    g_logits = (xf @ w_router_group + b_router_group).astype(jnp.float32)
    g_idx = jnp.argmax(g_logits, axis=-1)
    g_prob = jnp.take_along_axis(jax.nn.softmax(g_logits, axis=-1), g_idx[:, None], axis=1)[:, 0]
    e_logits = (jnp.einsum('nd,dge->nge', xf, w_router_expert) + b_router_expert).astype(jnp.float32)
    e_logits = jnp.take_along_axis(e_logits, g_idx[:, None, None], axis=1)[:, 0]
    top_v, top_i = lax.top_k(e_logits, TOPK_IN_GROUP)
    e_prob = jax.nn.softmax(top_v, axis=-1)
    expert_id = g_idx[:, None] * EXPERTS_PER_GROUP + top_i
    combine = g_prob[:, None] * jnp.sum(e_prob[..., None] * jax.nn.one_hot(expert_id, N_EXPERTS, dtype=jnp.float32), axis=1)
    y = jnp.zeros(xf.shape, jnp.float32)
    for e in range(N_EXPERTS):
        he = jax.nn.silu(xf @ w_gate[e]) * (xf @ w_up[e])
        y = y + combine[:, e:e + 1] * (he @ w_down[e])
    return y.reshape(B, T, D).astype(h.dtype)


def setup_inputs(seed: int = 0) -> dict:
    key = jax.random.key(seed)
    ks = jax.random.split(key, 20)
    f32 = jnp.float32
    nrm = lambda k, shape, s: s * jax.random.normal(k, shape, f32)
    L = DEPTH
    return {
        "x": jax.random.normal(ks[0], (BATCH, SEQ, D_MODEL), f32),
        "rel_bias": nrm(ks[1], (N_BUCKETS, N_HEADS), 0.1),
        "norm_mix": 1.0 + nrm(ks[2], (L, D_MODEL), 0.02),
        "w_in": nrm(ks[3], (L, D_MODEL, N_IN), D_MODEL ** -0.5),
        "w_out": nrm(ks[4], (L, D_MIX, D_MODEL), D_MIX ** -0.5),
        "cmp_pos_k": nrm(ks[5], (L, CMP_BLOCK, HEAD_DIM), 0.1),
        "cmp_pos_v": nrm(ks[6], (L, CMP_BLOCK, HEAD_DIM), 0.1),
        "cmp_k_w1": nrm(ks[7], (L, CMP_BLOCK * HEAD_DIM, CMP_HIDDEN), (CMP_BLOCK * HEAD_DIM) ** -0.5),
        "cmp_k_w2": nrm(ks[8], (L, CMP_HIDDEN, HEAD_DIM), CMP_HIDDEN ** -0.5),
        "cmp_v_w1": nrm(ks[9], (L, CMP_BLOCK * HEAD_DIM, CMP_HIDDEN), (CMP_BLOCK * HEAD_DIM) ** -0.5),
        "cmp_v_w2": nrm(ks[10], (L, CMP_HIDDEN, HEAD_DIM), CMP_HIDDEN ** -0.5),
        "norm_ffn": 1.0 + nrm(ks[11], (L, D_MODEL), 0.02),
        "w_router_group": nrm(ks[12], (L, D_MODEL, N_GROUPS), D_MODEL ** -0.5),
        "b_router_group": nrm(ks[13], (L, N_GROUPS), 0.01),
        "w_router_expert": nrm(ks[14], (L, D_MODEL, N_GROUPS, EXPERTS_PER_GROUP), D_MODEL ** -0.5),
        "b_router_expert": nrm(ks[15], (L, N_GROUPS, EXPERTS_PER_GROUP), 0.01),
        "w_gate": nrm(ks[16], (L, N_EXPERTS, D_MODEL, D_EXPERT), D_MODEL ** -0.5),
        "w_up": nrm(ks[17], (L, N_EXPERTS, D_MODEL, D_EXPERT), D_MODEL ** -0.5),
        "w_down": nrm(ks[18], (L, N_EXPERTS, D_EXPERT, D_MODEL), D_EXPERT ** -0.5),
        "norm_final": 1.0 + nrm(ks[19], (D_MODEL,), 0.02),
    }


def reference(x, rel_bias, norm_mix, w_in, w_out, cmp_pos_k, cmp_pos_v, cmp_k_w1, cmp_k_w2,
              cmp_v_w1, cmp_v_w2, norm_ffn, w_router_group, b_router_group, w_router_expert,
              b_router_expert, w_gate, w_up, w_down, norm_final):
    h = x
    for l in range(DEPTH):
        h = h + hybrid_mixer(rmsnorm(h, norm_mix[l]), w_in[l], w_out[l], cmp_pos_k[l], cmp_pos_v[l],
                             cmp_k_w1[l], cmp_k_w2[l], cmp_v_w1[l], cmp_v_w2[l], rel_bias)
        h = h + hier_moe(rmsnorm(h, norm_ffn[l]), w_router_group[l], b_router_group[l],
                         w_router_expert[l], b_router_expert[l], w_gate[l], w_up[l], w_down[l])
    return rmsnorm(h, norm_final)
```

```python
import math
import contextlib
import numpy as np
import concourse.bass as bass
import concourse.mybir as mybir
from concourse.bass_utils import run_bass_kernel_spmd

F32 = mybir.dt.float32
BF16 = mybir.dt.bfloat16
ALU = mybir.AluOpType
AF = mybir.ActivationFunctionType

D = 1024
NIN = 2590
NEG = -30000.0
EPS = 1e-6
RA = 20
RW = 8
NE = 16
DE = 512


class Sched:
    ENG = ["pe", "dve", "act", "pool", "sp"]

    def __init__(self, nc):
        self.nc = nc
        self.ops = {e: [] for e in self.ENG}
        self.last_w = {}
        self.readers = {}
        self.known = {e: {} for e in self.ENG}
        self.dma_sems = {}
        self.group = set()
        self.pending = {e: [] for e in self.ENG}

    def barrier(self):
        toks = []
        for e in self.ENG:
            for i in range(len(self.ops[e]) - 1, -1, -1):
                if self.ops[e][i]["dma"] is None:
                    toks.append(("E", e, i))
                    break
        for n, v in self.dma_sems.items():
            toks.append(("D", n, v[0]))
        for e in self.ENG:
            self.pending[e] = list(toks)

    mute = False

    def op(self, eng, fn, reads=(), writes=(), dma_sem=None, group=False):
        if self.mute:
            return None
        ops = self.ops[eng]
        idx = len(ops)
        deps = list(self.pending[eng])
        self.pending[eng] = []
        for r in reads:
            t = self.last_w.get(r)
            if t is not None:
                deps.append(t)
            if len(r) == 2 and r[0] == "B" and r[1].isdigit():
                for t2 in self.readers.get(r, ()):
                    if t2[0] == "E" and t2[1] != eng:
                        deps.append(t2)
        for w in writes:
            t = self.last_w.get(w)
            if t is not None:
                deps.append(t)
            deps.extend(self.readers.get(w, ()))
        if dma_sem is not None:
            ent = self.dma_sems.setdefault(dma_sem, [0])
            ent[0] += 16
            mytok = ("D", dma_sem, ent[0])
            if group:
                self.group.add(dma_sem)
        else:
            mytok = ("E", eng, idx)
        waits = []
        for t in deps:
            if t[0] == "E":
                if t[1] == eng and (eng == "pe" or self.ops[eng][t[2]]["dma"] is not None):
                    continue
                key = ("E", t[1])
            else:
                key = ("D", t[1])
            if self.known[eng].get(key, -1) >= t[2]:
                continue
            self.known[eng][key] = t[2]
            waits.append(t)
        rec = _Recorder()
        fn(rec)
        ops.append(dict(call=rec.call, waits=waits, dma=mytok if dma_sem else None, signal=False))
        for t in waits:
            if t[0] == "E":
                self.ops[t[1]][t[2]]["signal"] = True
        for r in reads:
            self.readers.setdefault(r, []).append(mytok)
        for w in writes:
            self.last_w[w] = mytok
            self.readers[w] = []
        return mytok

    def emit(self, final_wait_tokens=()):
        nc = self.nc
        with contextlib.ExitStack() as st:
            esem = {e: st.enter_context(nc.semaphore("s_" + e)) for e in self.ENG}
            dsem = {n: st.enter_context(nc.semaphore("d_" + n)) for n in self.dma_sems}
            sigval = {}
            for e in self.ENG:
                c = 0
                for i, o in enumerate(self.ops[e]):
                    if o["signal"]:
                        c += 1
                        sigval[(e, i)] = c
            block = st.enter_context(nc.Block())

            def do_wait(eng, t):
                if t[0] == "E":
                    eng.wait_ge(esem[t[1]], sigval[(t[1], t[2])])
                else:
                    v = self.dma_sems[t[1]][0] if t[1] in self.group else t[2]
                    eng.wait_ge(dsem[t[1]], v)

            def run(e, eng):
                for i, o in enumerate(self.ops[e]):
                    for t in o["waits"]:
                        do_wait(eng, t)
                    cname, cargs, ckw = o["call"]
                    ins = getattr(eng, cname)(*cargs, **ckw)
                    if o["dma"] is not None:
                        ins.then_inc(dsem[o["dma"][1]], 16)
                    elif o["signal"]:
                        ins.then_inc(esem[e], 1)
                if e == "sp":
                    for t in final_wait_tokens:
                        do_wait(eng, t)

            @block.tensor
            def _(eng):
                run("pe", eng)

            @block.vector
            def _(eng):
                run("dve", eng)

            @block.scalar
            def _(eng):
                run("act", eng)

            @block.gpsimd
            def _(eng):
                run("pool", eng)

            @block.sync
            def _(eng):
                run("sp", eng)


class _Recorder:
    def __init__(self):
        self.call = None

    def __getattr__(self, name):
        def f(*a, **k):
            self.call = (name, a, k)
            return None
        return f


class Rot:
    def __init__(self, name, bufs):
        self.name, self.bufs, self.i = name, bufs, 0

    def next(self):
        k = self.i % len(self.bufs)
        self.i += 1
        return self.bufs[k], "%s%d" % (self.name, k)


def _bucket(dist):
    d = np.maximum(dist, 0)
    large = 16 + (np.log(np.maximum(d, 1).astype(np.float32) / np.float32(16)) / np.float32(math.log(128.0))
                  * np.float32(16)).astype(np.int32)
    large = np.minimum(large, 31)
    return np.where(d < 16, d, large).astype(np.int64)


def _struct_tables(NB):
    NBX = NB + 2
    TX = NBX * 128
    NS = TX // 64
    NCX = TX // 16 - 1
    NCT = (NCX + 127) // 128
    ki = np.arange(128)[:, None, None]
    qi = np.arange(128)[None, None, :]
    dl = np.arange(14)[None, :, None]
    dist = 128 * dl + qi - ki
    idxN = np.where(dist < 0, 32, _bucket(dist))
    distw = 128 * 4 + qi - ki
    idxW = np.where((distw < 0) | (distw >= 512), 32, _bucket(distw))
    idxN = np.concatenate([idxN, idxW], axis=1)
    dl = np.arange(17)[None, :, None]
    dist = 128 * dl + qi - ki
    mult = ((dist >= 0) & (dist <= 128)).astype(np.int64) + ((dist >= 0) & (dist <= 512) & (dist % 4 == 0)) \
        + ((dist >= 0) & (dist <= 2048) & (dist % 16 == 0))
    idxA = np.where(mult == 0, 32, _bucket(dist))
    logm = np.log(np.maximum(mult, 1).astype(np.float64)).astype(np.float32)
    ni = np.arange(128)[:, None, None]
    ee = (2 * np.arange(15))[None, :, None]
    dist = 128 * ee + qi - 16 * ni - 31
    idxC = np.where(dist < 0, 32, _bucket(dist))
    n = np.arange(NCT * 128)[:, None]
    s = np.arange(NS)[None, :]
    ov = np.clip(np.minimum(16 * n + 32, 64 * s + 64) - np.maximum(16 * n, 64 * s), 0, None).astype(np.float32) / 32.0
    ov = ov.reshape(NCT, 128, NS).transpose(1, 0, 2).copy()
    EX = np.zeros((32, 16, 128), np.float32)
    for m in range(16):
        for key in range(128):
            EX[(2 * m) % 32 + key // 64, m, key] = 1.0
    E16 = np.zeros((32, 272), np.float32)
    for k in range(16):
        E16[k, 128 + k] = 1.0
    return dict(idxN=idxN, idxA=idxA, logm=logm, idxC=idxC, ov=ov, EX=EX, E16=E16)


def _core_tables(NB, p):
    NBX = NB + 2
    NSB = NBX // 2
    TX = NBX * 128
    NS = TX // 64
    NCX = TX // 16 - 1
    NCT = (NCX + 127) // 128
    n_cmp = (NB * 128 - 32) // 16 + 1
    keyvalid = np.full((128, NBX), NEG, np.float32)
    keyvalid[:, p:p + NB] = 0.0
    nn = np.arange(NCT * 128)
    cv = np.where((nn >= 8 * p) & (nn <= 8 * p + n_cmp - 1), 0.0, NEG).astype(np.float32)
    cmpvalid = cv.reshape(NCT, 128).T.copy()
    s0 = 2 * p
    s_last = s0 + NB * 2 - 1
    selmul = np.zeros((NSB, 128, NS), np.float32)
    seladd = np.zeros((NSB, 128, NS), np.float32)
    blk = np.arange(NS)[None, :]
    for sb in range(NSB):
        t = 256 * sb + np.arange(128)
        cur = (t // 64)[:, None]
        valid = (blk >= s0) & (blk <= cur) & (blk <= s_last)
        mul = valid.astype(np.float32)
        add = np.where(blk > cur, -1.0, 0.0)
        add = np.where((blk < s0) | (blk > s_last), -2.0, add)
        f3 = valid & (blk == s0)
        f2 = valid & (blk == cur - 1)
        f1 = valid & (blk == cur)
        for f, v in ((f3, 3.0e4), (f2, 2.0e4), (f1, 1.0e4)):
            add = np.where(f, v, add)
            mul = np.where(f, 0.0, mul)
        selmul[sb] = mul
        seladd[sb] = add
    return dict(keyvalid=keyvalid, cmpvalid=cmpvalid, selmul=selmul, seladd=seladd)


class _Stop(Exception):
    pass


def build_program(NB, G=9, debug=False, stop=None):
    NBX = NB + 2
    NSB = NBX // 2
    TX = NBX * 128
    NS = TX // 64
    NSG = (NS + 31) // 32
    NCX = TX // 16 - 1
    NCT = (NCX + 127) // 128
    assert 3 * NS <= 512

    nc = bass.Bass("TRN2", target_bir_lowering=False)

    def din(name, shape, dt=F32):
        return nc.dram_tensor(name, list(shape), dt, kind="ExternalInput").ap()

    xs = din("xs", [TX, D])
    w_in = din("w_in", [D, NIN])
    w_out = din("w_out", [D, D])
    gvec = din("gvec", [3, D])
    tabN_d = din("tabN", [2, 128, 15, 640])
    tabA_d = din("tabA", [128, 17, 768])
    logm_d = din("logm", [128, 17, 128])
    tabC_d = din("tabC", [15, 2, 128, 640])
    ov_d = din("ov", [128, NCT, NS])
    EX_d = din("EX", [32, 16 * 128])
    E16_d = din("E16", [32, 272])
    posT_d = din("posT", [2, 64, 32])
    w1_d = din("w1", [2, 2048, 256])
    w2_d = din("w2", [2, 256, 64])
    keyvalid_d = din("keyvalid", [128, NBX])
    cmpvalid_d = din("cmpvalid", [128, NCT])
    selmul_d = din("selmul", [NSB, 128, NS])
    seladd_d = din("seladd", [NSB, 128, NS])
    wr_d = din("wr", [D, 20])
    br_d = din("br", [1, 20])
    wg_d = din("wg", [NE, D, DE])
    wu_d = din("wu", [NE, D, DE])
    wd_d = din("wd", [NE, DE, D])
    out_d = nc.dram_tensor("out", [NSB * 128, D], F32, kind="ExternalOutput").ap()
    o_scr = nc.dram_tensor("o_scr", [NSB * 128, D], BF16, kind="ExternalOutput" if debug else "Internal").ap()

    S = Sched(nc)
    op = S.op
    with contextlib.ExitStack() as top:
        _uid = [0]

        def sbt(st, name, shape, dt):
            _uid[0] += 1
            return st.enter_context(nc.sbuf_tensor("sb%d_%s" % (_uid[0], name), list(shape), dt))

        ps = top.enter_context(nc.psum_tensor("ps", [128, 4096], F32))

        def bk(b, lo=0, hi=512):
            return ps[:, b * 512 + lo: b * 512 + hi]

        def bkb(b):
            return ps[:, b * 512:(b + 1) * 512].bitcast(BF16)

        identf = sbt(top, "identf", [128, 128], F32)
        identb = sbt(top, "identb", [128, 128], BF16)
        gbc = sbt(top, "gbc", [128, 3, D], F32)
        epsc = sbt(top, "epsc", [128, 1], F32)
        keyvalid = sbt(top, "keyvalid", [128, NBX], F32)
        cmpvalid = sbt(top, "cmpvalid", [128, NCT], F32)
        kcmpT = sbt(top, "kcmpT", [128, NCT * 128], BF16)
        vcmp = sbt(top, "vcmp", [128, NCT, 2, 65], BF16)
        stg = sbt(top, "stg", [128, 2048], F32)

        op("pool", lambda e: e.memset(identf[:], 1.0), writes=["identf"])
        op("pool", lambda e: e.affine_select(out=identf[:], in_=identf[:], pattern=[[1, 128]], compare_op=ALU.is_equal,
                                             fill=0.0, base=0, channel_multiplier=-1), reads=["identf"], writes=["identf"])
        op("pool", lambda e: e.tensor_copy(out=identb[:], in_=identf[:]), reads=["identf"], writes=["identb"])
        op("pool", lambda e: e.memset(epsc[:], EPS), writes=["epsc"])
        op("pool", lambda e: e.memset(kcmpT[:], 0.0), writes=["kcmpT"])
        op("pool", lambda e: e.memset(vcmp[:], 0.0), writes=["vcmp"])
        op("pool", lambda e: e.memset(vcmp[:, :, :, 64:65], 1.0), reads=["vcmp"], writes=["vcmp"])
        op("sp", lambda e: e.dma_start(out=gbc[:].rearrange("p a d -> p (a d)"),
                                       in_=gvec.rearrange("a d -> (a d)").partition_broadcast(128)), writes=["gbc"], dma_sem="cst", group=True)
        op("sp", lambda e: e.dma_start(out=keyvalid[:], in_=keyvalid_d), writes=["keyvalid"], dma_sem="cst", group=True)
        op("sp", lambda e: e.dma_start(out=cmpvalid[:], in_=cmpvalid_d), writes=["cmpvalid"], dma_sem="cst", group=True)

        def load_cast(src_ap, dst_ap, n, eng="dve", stg_view=None):
            sv = stg[:, 0:n] if stg_view is None else stg_view
            op("sp", lambda e, s=src_ap, v=sv: e.dma_start(out=v, in_=s), writes=["stg"], dma_sem="stg")
            op(eng, lambda e, v=sv, d=dst_ap: e.tensor_copy(out=d, in_=v), reads=["stg"], writes=["wcast"])

        def rmsnorm_block(xin_ap, xin_reg, gi, out_ap, out_reg, ssq_ap, tmp_ap, junk_ap, tag):
            op("dve", lambda e: e.memset(ssq_ap, 0.0), writes=["st_" + tag])
            op("act", lambda e: e.activation(out=junk_ap, in_=xin_ap, func=AF.Square, accum_out=ssq_ap),
               reads=[xin_reg, "st_" + tag], writes=["junk_" + tag, "st_" + tag])
            op("act", lambda e: e.activation(out=tmp_ap, in_=ssq_ap, func=AF.Sqrt, bias=epsc[:], scale=1.0 / D),
               reads=["st_" + tag, "epsc"], writes=["st_" + tag])
            op("dve", lambda e: e.reciprocal(out=tmp_ap, in_=tmp_ap), reads=["st_" + tag], writes=["st_" + tag])
            op("dve", lambda e: e.scalar_tensor_tensor(out=out_ap, in0=xin_ap, scalar=tmp_ap, in1=gbc[:, gi, :],
                                                       op0=ALU.mult, op1=ALU.mult),
               reads=[xin_reg, "st_" + tag, "gbc"], writes=[out_reg])

        tpbank = [7]

        def transpose_block(src_ap_fn, src_reg, dst_ap, dst_reg, nch, bank, fp32=False, eng="act"):
            breg = "B%d" % bank
            if fp32:
                for k in range(nch):
                    op("pe", lambda e, k=k: e.transpose(out=bk(bank, k * 128, (k + 1) * 128), in_=src_ap_fn(k), identity=identf[:]),
                       reads=[src_reg, "identf"], writes=[breg])
                src = bk(bank, 0, nch * 128).rearrange("p (k t) -> p k t", k=nch)
            else:
                for k in range(nch):
                    op("pe", lambda e, k=k: e.transpose(out=bkb(bank)[:, k * 128:(k + 1) * 128], in_=src_ap_fn(k), identity=identb[:]),
                       reads=[src_reg, "identb"], writes=[breg])
                src = bkb(bank)[:, 0:nch * 128].rearrange("p (k t) -> p k t", k=nch)
            if eng == "act":
                op("act", lambda e: e.copy(out=dst_ap, in_=src), reads=[breg], writes=[dst_reg])
            else:
                op("dve", lambda e: e.tensor_copy(out=dst_ap, in_=src), reads=[breg], writes=[dst_reg])

        evac_flip = [0]

        def evac(dst_ap, src_ap, reads, writes, scale=None, eng=None):
            if eng is None:
                evac_flip[0] ^= 1
                use_act = bool(evac_flip[0])
            else:
                use_act = (eng == "act")
            if use_act:
                if scale is None:
                    op("act", lambda e: e.copy(out=dst_ap, in_=src_ap), reads=reads, writes=writes)
                else:
                    op("act", lambda e: e.mul(out=dst_ap, in_=src_ap, mul=scale), reads=reads, writes=writes)
            else:
                if scale is None:
                    op("dve", lambda e: e.tensor_copy(out=dst_ap, in_=src_ap), reads=reads, writes=writes)
                else:
                    op("dve", lambda e: e.tensor_scalar(out=dst_ap, in0=src_ap, scalar1=scale, scalar2=None, op0=ALU.mult),
                       reads=reads, writes=writes)

        def load_norm_transpose(st_bufs, sb, nblk, gi):
            (xt, xn, xnT, ssq, tmp, junk) = st_bufs["cur"]()
            xt_t, xt_reg = xt
            xnT_t, xnT_reg = xnT
            op("sp", lambda e: e.dma_start(out=xt_t[:, 0:nblk, :], in_=xs[sb * 256: sb * 256 + nblk * 128, :].rearrange("(b p) d -> p b d", p=128)),
               writes=[xt_reg], dma_sem=xt_reg)
            for b in range(nblk):
                rmsnorm_block(xt_t[:, b, :], xt_reg, gi, xn[:, b, :], "xn", ssq[:, b:b + 1], tmp[:, b:b + 1], junk[:], "n%d" % b)
                transpose_block(lambda k, b=b: xn[:, b, k * 128:(k + 1) * 128], "xn", xnT_t[:, :, b * 128:(b + 1) * 128], xnT_reg, 8, 7,
                                eng="act" if b == 0 else "dve")
            return xt_t, xt_reg, xnT_t, xnT_reg

        pjbanks = Rot("B", [0, 1, 2, 3])

        def proj_fm(w_ap_fn, xnT_t, xnT_reg, ntok, dst_ap, dst_reg, wreg, scale=None):
            b = pjbanks.bufs[pjbanks.i % 4]
            pjbanks.i += 1
            breg = "B%d" % b
            for kc in range(8):
                op("pe", lambda e, kc=kc: e.matmul(bk(b, 0, ntok), lhsT=w_ap_fn(kc), rhs=xnT_t[:, kc, 0:ntok], start=(kc == 0), stop=(kc == 7)),
                   reads=[wreg, xnT_reg], writes=[breg])
            evac(dst_ap, bk(b, 0, ntok), [breg], [dst_reg], scale)

        def proj_tm(xnT_t, xnT_reg, blk, w_ap_fn, ncols, wreg):
            b = pjbanks.bufs[pjbanks.i % 4]
            pjbanks.i += 1
            breg = "B%d" % b
            for kc in range(8):
                op("pe", lambda e, kc=kc: e.matmul(bk(b, 0, ncols), lhsT=xnT_t[:, kc, blk * 128:(blk + 1) * 128], rhs=w_ap_fn(kc),
                                                   start=(kc == 0), stop=(kc == 7)),
                   reads=[wreg, xnT_reg], writes=[breg])
            return bk(b, 0, ncols), breg

        def make_xbufs(st):
            xts = [sbt(st, "xt%d" % i, [128, 2, D], F32) for i in range(2)]
            xnTs = [sbt(st, "xnT%d" % i, [128, 8, 256], BF16) for i in range(2)]
            xn = sbt(st, "xn", [128, 2, D], BF16)
            ssq = sbt(st, "ssq", [128, 2], F32)
            tmp = sbt(st, "tmpn", [128, 2], F32)
            junk = sbt(st, "junk", [128, D], BF16)
            cnt = [0]

            def cur():
                k = cnt[0] % 2
                cnt[0] += 1
                return ((xts[k], "xt%d" % k), xn, (xnTs[k], "xnT%d" % k), ssq, tmp, junk)
            return {"cur": cur}

        def chk(name):
            if stop == name:
                S.mute = True

        def pass_A():
          with contextlib.ExitStack() as st:
            winA = sbt(st, "winA", [128, 8, 1408], BF16)
            w1s = sbt(st, "w1s", [128, 2, 32, 256], BF16)
            w2s = sbt(st, "w2s", [128, 2, 2, 128], BF16)
            posT = sbt(st, "posT", [128, 2, 32], BF16)
            c1 = sbt(st, "c1", [128, 4], F32)
            tabA = sbt(st, "tabA", [128, 17, 768], BF16)
            E16 = sbt(st, "E16", [32, 272], BF16)
            kAT = sbt(st, "kAT", [128, 3, RA * 128], BF16)
            vA = sbt(st, "vA", [128, RA, 6, 65], BF16)
            kct = [sbt(st, "kct%d" % i, [128, 2, 272], BF16) for i in range(2)]
            qAT = sbt(st, "qAT", [128, 3, 128], BF16)
            PTs = [sbt(st, "PTa%d" % i, [128, 768], BF16) for i in range(3)]
            kgath = sbt(st, "kgath", [128, 2, 32, 16], BF16)
            h1u = sbt(st, "h1u", [128, 128], F32)
            h1a = sbt(st, "h1a", [128, 128], F32)
            h1b = sbt(st, "h1b", [128, 128], F32)
            h1g = sbt(st, "h1g", [128, 160], BF16)
            vnew = sbt(st, "vnew", [32, 2, 64], BF16)
            rdenA = sbt(st, "rdenA", [128, 6], F32)
            oA = [sbt(st, "oA%d" % i, [128, 384], BF16) for i in range(2)]
            xb = make_xbufs(st)

            for kc in range(8):
                load_cast(w_in[kc * 128:(kc + 1) * 128, 0:1152], winA[:, kc, 0:1152], 1152, eng="dve")
                load_cast(w_in[kc * 128:(kc + 1) * 128, 1792:2048], winA[:, kc, 1152:1408], 256, eng="pool")
            for kv in range(2):
                for lc in range(4):
                    for half in range(2):
                        sv = stg[half * 64:(half + 1) * 64, :]
                        src = w1_d[kv, lc * 512:(lc + 1) * 512, :].rearrange("(l d) h -> d l h", d=64)
                        dst = w1s[half * 64:(half + 1) * 64, kv, lc * 8:(lc + 1) * 8, :]
                        op("sp", lambda e, s=src, v=sv: e.dma_start(out=v.rearrange("p (l h) -> p l h", l=8), in_=s), writes=["stg"], dma_sem="stg")
                        op("dve" if half else "pool", lambda e, v=sv, d=dst: e.tensor_copy(out=d, in_=v.rearrange("p (l h) -> p l h", l=8)),
                           reads=["stg"], writes=["wcast"])
                for dup in range(2):
                    load_cast(w2_d[kv].rearrange("(c p) d -> p c d", p=128), w2s[:, kv, :, dup * 64:(dup + 1) * 64], 128, stg_view=stg[:, 0:128].rearrange("p (c d) -> p c d", c=2))
                for half in range(2):
                    load_cast(posT_d[kv], posT[half * 64:(half + 1) * 64, kv, :], 32, stg_view=stg[half * 64:(half + 1) * 64, 0:32])
            load_cast(E16_d, E16[:], 272, stg_view=stg[0:32, 0:272])
            for dl in range(17):
                op("sp", lambda e, dl=dl: e.dma_start(out=stg[:, 0:768], in_=tabA_d[:, dl, :]), writes=["stg"], dma_sem="stg")
                op("sp", lambda e, dl=dl: e.dma_start(out=stg[:, 768:896], in_=logm_d[:, dl, :]), writes=["stg2"], dma_sem="stg2")
                op("dve", lambda e, dl=dl: e.tensor_tensor(out=tabA[:, dl, :].rearrange("p (h q) -> p h q", h=6),
                                                           in0=stg[:, 0:768].rearrange("p (h q) -> p h q", h=6),
                                                           in1=stg[:, 768:896].unsqueeze(1).to_broadcast([128, 6, 128]), op=ALU.add),
                   reads=["stg", "stg2"], writes=["wcast"])
            op("pool", lambda e: e.memset(vA[:, :, :, 64:65], 1.0), writes=["vA%d" % s_ for s_ in range(0, RA, 2)])
            op("pool", lambda e: e.memset(h1u[:], 0.0), writes=["h1u"])
            op("pool", lambda e: e.memset(h1g[:], 0.0), writes=["h1g"])
            op("pool", lambda e: e.memset(kct[0][:], 0.0), writes=["kct0"])
            op("pool", lambda e: e.memset(kct[1][:], 0.0), writes=["kct1"])
            for kv in range(2):
                for hc in range(2):
                    col = kv * 2 + hc
                    for l in range(32):
                        op("pe", lambda e, kv=kv, hc=hc, l=l, col=col: e.matmul(bk(6, col, col + 1), lhsT=w1s[0:64, kv, l, hc * 128:(hc + 1) * 128],
                                                                                 rhs=posT[0:64, kv, l:l + 1], start=(l == 0), stop=(l == 31)),
                           reads=["wcast"], writes=["B6"])
            op("dve", lambda e: e.tensor_copy(out=c1[:], in_=bk(6, 0, 4)), reads=["B6"], writes=["c1"])

            sbufs = Rot("SA", [0, 2])
            for sb in range(0 if stop == "A_init" else NSB):
                if sb == 1 and stop == "A_1":
                    break
                j = 2 * sb
                xt_t, xt_reg, xnT_t, xnT_reg = load_norm_transpose(xb, sb, 2, 0)
                chk("A_1a")
                slot0 = j % RA
                for pr in range(3):
                    proj_fm(lambda kc, pr=pr: winA[:, kc, 384 + pr * 128: 384 + (pr + 1) * 128], xnT_t, xnT_reg, 256,
                            kAT[:, pr, slot0 * 128: slot0 * 128 + 256], "kA%d" % slot0, "wcast")
                kc_t = kct[sb % 2]
                kc_reg = "kct%d" % (sb % 2)
                kp_t = kct[(sb + 1) % 2]
                kp_reg = "kct%d" % ((sb + 1) % 2)
                if sb > 0:
                    op("pool", lambda e, a=kc_t, b=kp_t: e.tensor_copy(out=a[:, :, 0:16], in_=b[:, :, 256:272]), reads=[kp_reg], writes=[kc_reg])
                for kv in range(2):
                    proj_fm(lambda kc, kv=kv: winA[:, kc, 1152 + kv * 128: 1152 + (kv + 1) * 128], xnT_t, xnT_reg, 256,
                            kc_t[:, kv, 16:272], kc_reg, "wcast")
                for pr in range(3):
                    proj_fm(lambda kc, pr=pr: winA[:, kc, pr * 128:(pr + 1) * 128], xnT_t, xnT_reg, 128, qAT[:, pr, :], "qAT", "wcast", scale=0.125)
                for b in range(2):
                    pv, breg = proj_tm(xnT_t, xnT_reg, b, lambda kc: winA[:, kc, 768:1152], 384, "wcast")
                    evac(vA[:, slot0 + b, :, 0:64], pv.rearrange("p (h d) -> p h d", h=6), [breg], ["vA%d" % slot0])
                chk("A_1b")
                i0 = 0
                cnt = 16
                nfirst = 16 * sb - 1
                assert nfirst + cnt <= NCX
                if cnt > 0:
                    for kv in range(2):
                        op("dve", lambda e, kv=kv: e.tensor_copy(out=kgath[:, kv, :, :], in_=kc_t[:, kv, :].rearrange("p (n l) -> p l n", l=16)[:, :, 0:16]) if False else
                           e.tensor_copy(out=kgath[:, kv, 0:16, :], in_=kc_t[:, kv, 0:256].rearrange("p (n l) -> p l n", l=16)),
                           reads=[kc_reg], writes=["kgath"])
                        op("dve", lambda e, kv=kv: e.tensor_copy(out=kgath[:, kv, 16:32, :], in_=kc_t[:, kv, 16:272].rearrange("p (n l) -> p l n", l=16)),
                           reads=[kc_reg], writes=["kgath"])
                    chk("A_cg")
                    for kv in range(2):
                        for kvh in range(2):
                            rows = slice(kvh * 64, (kvh + 1) * 64)
                            for hc in range(2):
                                grp = (kv * 2 + kvh) * 2 + hc
                                for l in range(32):
                                    hb = 6 if kvh == 0 else 4
                                    hoff = 0 if kvh == 0 else 256
                                    op("pe", lambda e, kv=kv, rows=rows, hc=hc, grp=grp, l=l, hb=hb, hoff=hoff: e.matmul(
                                        bk(hb, hoff + grp * 16, hoff + grp * 16 + cnt), lhsT=w1s[rows, kv, l, hc * 128:(hc + 1) * 128],
                                        rhs=kgath[rows, kv, l, :], start=(l == 0), stop=(l == 31)),
                                       reads=["wcast", "kgath"], writes=["B%d" % hb])
                    chk("A_c0")
                    for grp in range(8):
                        kv, hc = grp // 4, grp % 2
                        kvh_ = (grp // 2) % 2
                        hb = 6 if kvh_ == 0 else 4
                        hoff = 0 if kvh_ == 0 else 256
                        op("dve", lambda e, grp=grp, kv=kv, hc=hc, hb=hb, hoff=hoff: e.tensor_scalar(out=h1u[:, grp * 16: grp * 16 + cnt], in0=bk(hb, hoff + grp * 16, hoff + grp * 16 + cnt),
                                                                                    scalar1=c1[:, kv * 2 + hc: kv * 2 + hc + 1], scalar2=None, op0=ALU.add),
                           reads=["B%d" % hb, "c1"], writes=["h1u"])
                    chk("A_c1")
                    op("dve", lambda e: e.tensor_tensor(out=h1a[:], in0=h1u[:], in1=h1u[:], op=ALU.mult), reads=["h1u"], writes=["h1a"])
                    op("dve", lambda e: e.tensor_scalar(out=h1a[:], in0=h1a[:], scalar1=0.044715, scalar2=1.0, op0=ALU.mult, op1=ALU.add), reads=["h1a"], writes=["h1a"])
                    op("dve", lambda e: e.tensor_tensor(out=h1a[:], in0=h1a[:], in1=h1u[:], op=ALU.mult), reads=["h1a", "h1u"], writes=["h1a"])
                    op("act", lambda e: e.activation(out=h1b[:], in_=h1a[:], func=AF.Exp, scale=-1.5957691216057308), reads=["h1a"], writes=["h1b"])
                    op("dve", lambda e: e.tensor_scalar(out=h1b[:], in0=h1b[:], scalar1=1.0, scalar2=None, op0=ALU.add), reads=["h1b"], writes=["h1b"])
                    op("dve", lambda e: e.reciprocal(out=h1b[:], in_=h1b[:]), reads=["h1b"], writes=["h1b"])
                    op("dve", lambda e: e.tensor_tensor(out=h1g[:, 0:128], in0=h1u[:], in1=h1b[:], op=ALU.mult), reads=["h1u", "h1b"], writes=["h1g"])
                    chk("A_c2")
                    for kvh in range(2):
                        for hc in range(2):
                            grp = (1 * 2 + kvh) * 2 + hc
                            op("pe", lambda e, kvh=kvh, hc=hc, grp=grp: e.matmul(ps[0:32, 5 * 512 + 64 + kvh * 64: 5 * 512 + 64 + (kvh + 1) * 64],
                                                                                 lhsT=h1g[:, grp * 16: grp * 16 + 32], rhs=w2s[:, 1, hc, 0:64],
                                                                                 start=(hc == 0), stop=(hc == 1)),
                               reads=["wcast", "h1g"], writes=["B5"])
                    op("dve", lambda e: e.tensor_copy(out=vnew[:, :, :], in_=ps[0:32, 5 * 512 + 64: 5 * 512 + 192].rearrange("p (k d) -> p k d", k=2)),
                       reads=["B5"], writes=["vnew"])
                    chk("A_c3")
                    c_lo = nfirst // 128
                    c_hi = (nfirst + cnt - 1) // 128
                    for c in range(max(c_lo, 0), c_hi + 1):
                        base = nfirst - 128 * c
                        op("pe", lambda e, base=base: e.matmul(bk(4, 0, 128), lhsT=E16[:, 128 - base: 256 - base],
                                                               rhs=vnew[:, :, :].rearrange("p k d -> p (k d)"), start=True, stop=True),
                           reads=["vnew", "wcast"], writes=["B4"])
                        op("dve", lambda e, c=c: e.tensor_tensor(out=vcmp[:, c, :, 0:64], in0=vcmp[:, c, :, 0:64],
                                                                 in1=bk(4, 0, 128).rearrange("p (k d) -> p k d", k=2), op=ALU.add),
                           reads=["B4", "vcmp"], writes=["vcmp"])
                    chk("A_c4")
                    for kvh in range(2):
                        for hc in range(2):
                            grp = (0 * 2 + kvh) * 2 + hc
                            op("pe", lambda e, kvh=kvh, hc=hc, grp=grp: e.matmul(bk(4, 448 + kvh * 16, 448 + kvh * 16 + cnt),
                                                                                 lhsT=w2s[:, 0, hc, :],
                                                                                 rhs=h1g[:, grp * 16: grp * 16 + cnt], start=(hc == 0), stop=(hc == 1)),
                               reads=["wcast", "h1g"], writes=["B4"])
                    for kvh in range(2):
                        rows = slice(kvh * 64, (kvh + 1) * 64)
                        d0 = 1 if nfirst < 0 else 0
                        op("dve", lambda e, kvh=kvh, rows=rows, d0=d0: e.tensor_copy(out=kcmpT[rows, nfirst + d0: nfirst + cnt],
                                                                                     in_=ps[rows, 4 * 512 + 448 + kvh * 16 + d0: 4 * 512 + 448 + kvh * 16 + cnt]),
                           reads=["B4"], writes=["kcmpT"])

                chk("A_1c")
                kb0 = max(0, j - 16)
                pendA = [None]
                for kb in range(kb0, j + 1):
                    dl = j - kb
                    slot = kb % RA
                    sreg_slot = "kA%d" % (slot - slot % 2)
                    vreg_slot = "vA%d" % (slot - slot % 2)
                    sbk, _ = sbufs.next()
                    sregs = ["B%d" % sbk, "B%d" % (sbk + 1)]
                    for h in range(6):
                        rows = slice((h % 2) * 64, (h % 2) * 64 + 64)
                        outap = ps[:, (sbk + h % 2) * 512 + (h // 2) * 128: (sbk + h % 2) * 512 + (h // 2 + 1) * 128]
                        op("pe", lambda e, h=h, rows=rows, outap=outap, slot=slot: e.matmul(outap, lhsT=kAT[rows, h // 2, slot * 128:(slot + 1) * 128],
                                                                                             rhs=qAT[rows, h // 2, :], start=True, stop=False),
                           reads=[sreg_slot, "qAT"], writes=sregs)
                        op("pe", lambda e, h=h, outap=outap, dl=dl: e.matmul(outap, lhsT=identb[:], rhs=tabA[:, dl, h * 128:(h + 1) * 128], start=False, stop=True),
                           reads=["identb", "wcast"], writes=sregs)
                    PT, ptreg = None, None
                    ptb = PTs[(kb - kb0) % 3]
                    ptreg = "PTa%d" % ((kb - kb0) % 3)
                    op("act", lambda e, sbk=sbk, ptb=ptb, kb=kb: e.activation(out=ptb[:].rearrange("p (a c) -> p a c", a=2), in_=ps[:, sbk * 512: sbk * 512 + 1024].rearrange("p (a c) -> p a c", a=2)[:, :, 0:384], func=AF.Exp,
                                                                               bias=keyvalid[:, kb:kb + 1]),
                       reads=sregs + ["keyvalid"], writes=[ptreg])
                    def mk_pv(ptb=ptb, ptreg=ptreg, slot=slot, kb=kb, vreg_slot=vreg_slot):
                        def f():
                            for h in range(6):
                                op("pe", lambda e, h=h: e.matmul(bk(5, h * 65, h * 65 + 65), lhsT=ptb[:, (h % 2) * 384 + (h // 2) * 128: (h % 2) * 384 + (h // 2 + 1) * 128],
                                                                 rhs=vA[:, slot, h, :], start=(kb == kb0 and h == 0), stop=(kb == j), skip_group_check=True),
                                   reads=[ptreg, vreg_slot], writes=["B5"])
                        return f
                    if pendA[0] is not None:
                        pendA[0]()
                    pendA[0] = mk_pv()
                if pendA[0] is not None:
                    pendA[0]()
                    pendA[0] = None
                op("dve", lambda e: e.tensor_scalar(out=rdenA[:], in0=bk(5, 0, 390).rearrange("p (h d) -> p h d", h=6)[:, :, 64], scalar1=1e-30, scalar2=None, op0=ALU.max),
                   reads=["B5"], writes=["rdenA"])
                op("dve", lambda e: e.reciprocal(out=rdenA[:], in_=rdenA[:]), reads=["rdenA"], writes=["rdenA"])
                oAb = oA[sb % 2]
                oreg = "oA%d" % (sb % 2)
                op("dve", lambda e, oAb=oAb: e.tensor_tensor(out=oAb[:].rearrange("p (h d) -> p h d", h=6),
                                                            in0=bk(5, 0, 390).rearrange("p (h d) -> p h d", h=6)[:, :, 0:64],
                                                            in1=rdenA[:].unsqueeze(2).to_broadcast([128, 6, 64]), op=ALU.mult),
                   reads=["B5", "rdenA"], writes=[oreg])
                op("pool", lambda e, oAb=oAb, sb=sb: e.dma_start(out=o_scr[sb * 128:(sb + 1) * 128, 0:384], in_=oAb[:]),
                   reads=[oreg], writes=["oscr%d" % sb], dma_sem=oreg)

        def pass_B():
          with contextlib.ExitStack() as st:
            winB = sbt(st, "winB", [128, 8, 1184], BF16)
            tabN = [sbt(st, "tabN%d" % k, [128, 15, 640], BF16) for k in range(2)]
            EXs = sbt(st, "EXs", [32, 16 * 128], BF16)
            ovs = sbt(st, "ovs", [128, NCT, NS], BF16)
            ksT = sbt(st, "ksT", [128, TX], BF16)
            vs = sbt(st, "vs", [128, NBX, 2, 65], BF16)
            kwT = sbt(st, "kwT", [128, RW * 128], BF16)
            vw = sbt(st, "vw", [128, RW, 2, 65], BF16)
            qT = sbt(st, "qT", [128, 5, 128], BF16)
            gsig = sbt(st, "gsig", [128, 30], F32)
            cb = [sbt(st, "cb%d" % i, [128, 640], F32) for i in range(2)]
            scs = sbt(st, "scs", [128, 640], F32)
            PTs = [sbt(st, "PTb%d" % i, [128, 640], BF16) for i in range(3)]
            PT2 = [sbt(st, "PT2b%d" % i, [128, 640], BF16) for i in range(2)]
            smul = [sbt(st, "smul%d" % i, [128, NS], F32) for i in range(2)]
            sadd = [sbt(st, "sadd%d" % i, [128, NS], F32) for i in range(2)]
            NS8 = ((NS + 7) // 8) * 8 + 8
            imp = sbt(st, "imp", [128, NS8], F32)
            impw = sbt(st, "impw", [128, NS8], F32)
            mx = sbt(st, "mx", [128, 16], F32)
            selm = sbt(st, "selm", [128, NSG * 32], BF16)
            selT = sbt(st, "selT", [32, NSG, 128], BF16)
            rden = sbt(st, "rden", [128, 3, 5], F32)
            coef = sbt(st, "coef", [128, 3, 5], F32)
            ob = sbt(st, "ob", [128, 320], F32)
            ob2 = sbt(st, "ob2", [128, 320], F32)
            oB = [sbt(st, "oB%d" % i, [128, 640], BF16) for i in range(2)]
            xb = make_xbufs(st)

            op("pool", lambda e: e.memset(winB[:, :, 1182:1184], 0.0), writes=["wcast"])
            for kc in range(8):
                op("sp", lambda e, kc=kc: e.dma_start(out=stg[:, 0:640], in_=w_in[kc * 128:(kc + 1) * 128, 1152:1792]), writes=["stg"], dma_sem="stg")
                op("dve", lambda e, kc=kc: e.tensor_copy(out=winB[:, kc, 0:640].rearrange("p (g k d) -> p k g d", g=5, k=2),
                                                         in_=stg[:, 0:640].rearrange("p (k g d) -> p k g d", k=2, g=5)), reads=["stg"], writes=["wcast"])
                op("sp", lambda e, kc=kc: e.dma_start(out=stg[:, 0:542], in_=w_in[kc * 128:(kc + 1) * 128, 2048:2590]), writes=["stg"], dma_sem="stg")
                for (so, do) in ((0, 640), (256, 768), (128, 896), (384, 1024)):
                    op("pool", lambda e, kc=kc, so=so, do=do: e.tensor_copy(out=winB[:, kc, do:do + 128], in_=stg[:, so:so + 128]), reads=["stg"], writes=["wcast"])
                op("pool", lambda e, kc=kc: e.tensor_copy(out=winB[:, kc, 1152:1182], in_=stg[:, 512:542]), reads=["stg"], writes=["wcast"])
            for k in range(2):
                for dl in range(15):
                    load_cast(tabN_d[k, :, dl, :], tabN[k][:, dl, :], 640, eng="dve" if dl % 2 else "pool")
            load_cast(EX_d, EXs[:], 2048, stg_view=stg[0:32, 0:2048])
            load_cast(ov_d.rearrange("p c s -> p (c s)"), ovs[:].rearrange("p c s -> p (c s)"), NCT * NS)
            op("pool", lambda e: e.memset(vs[:, :, :, 64:65], 1.0), writes=["vs%d" % s_ for s_ in range(NSB)])
            op("pool", lambda e: e.memset(vw[:, :, :, 64:65], 1.0), writes=["vw%d" % s_ for s_ in range(0, RW, 2)])
            op("pool", lambda e: e.memset(selm[:], 0.0), writes=["selm"])
            op("pool", lambda e: e.memset(imp[:], -1.0e30), writes=["imp"])

            sbufs = Rot("SB", [0, 2])
            for sb in range(0 if stop == "B_init" else NSB):
                if sb == 1 and stop == "B_1":
                    break
                j = 2 * sb
                xt_t, xt_reg, xnT_t, xnT_reg = load_norm_transpose(xb, sb, 2, 0)
                wslot0 = j % RW
                sm_t, sm_reg = smul[sb % 2], "smul%d" % (sb % 2)
                sa_t, sa_reg = sadd[sb % 2], "sadd%d" % (sb % 2)
                op("sp", lambda e, sb=sb, t=sm_t: e.dma_start(out=t[:], in_=selmul_d[sb]), writes=[sm_reg], dma_sem=sm_reg)
                op("sp", lambda e, sb=sb, t=sa_t: e.dma_start(out=t[:], in_=seladd_d[sb]), writes=[sa_reg], dma_sem=sa_reg)
                chk("B_p0")
                proj_fm(lambda kc: winB[:, kc, 640:768], xnT_t, xnT_reg, 256, ksT[:, j * 128: j * 128 + 256], "ksT%d" % sb, "wcast")
                proj_fm(lambda kc: winB[:, kc, 768:896], xnT_t, xnT_reg, 256, kwT[:, wslot0 * 128: wslot0 * 128 + 256], "kw%d" % wslot0, "wcast")
                chk("B_p1")
                for g in range(5):
                    proj_fm(lambda kc, g=g: winB[:, kc, g * 128:(g + 1) * 128], xnT_t, xnT_reg, 128, qT[:, g, :], "qT", "wcast", scale=0.125)
                chk("B_p2")
                for b in range(2):
                    pv, breg = proj_tm(xnT_t, xnT_reg, b, lambda kc: winB[:, kc, 896:1152], 256, "wcast")
                    ve = "act" if b == 0 else "dve"
                    evac(vs[:, j + b, :, 0:64], pv[:, 0:128].rearrange("p (k d) -> p k d", k=2), [breg], ["vs%d" % sb], eng=ve)
                    evac(vw[:, wslot0 + b, :, 0:64], pv[:, 128:256].rearrange("p (k d) -> p k d", k=2), [breg], ["vw%d" % wslot0], eng=ve)
                    if b == 0:
                        chk("B_p3")
                        pg, greg = proj_tm(xnT_t, xnT_reg, 0, lambda kc: winB[:, kc, 1152:1184], 32, "wcast")
                        op("act", lambda e, pg=pg: e.activation(out=gsig[:], in_=pg[:, 0:30], func=AF.Exp, scale=-1.0), reads=[greg], writes=["gsig"])
                        op("dve", lambda e: e.tensor_scalar(out=gsig[:], in0=gsig[:], scalar1=1.0, scalar2=None, op0=ALU.add), reads=["gsig"], writes=["gsig"])
                        op("dve", lambda e: e.reciprocal(out=gsig[:], in_=gsig[:]), reads=["gsig"], writes=["gsig"])

                chk("B_p")
                oBb = oB[sb % 2]
                oreg = "oB%d" % (sb % 2)
                for kvh in range(2):
                    rows = slice(kvh * 64, (kvh + 1) * 64)
                    qrhs = qT[rows, :, :].rearrange("p g q -> p (g q)")
                    gk = gsig[:, kvh * 15:(kvh + 1) * 15].rearrange("p (g r) -> p g r", r=3)

                    def scores(sbk, lhsT_ap, lreads, tab_ap, sregs):
                        for (lo, hi) in ((0, 512), (512, 640)):
                            outap = ps[:, sbk * 512 + lo: sbk * 512 + hi]
                            op("pe", lambda e, outap=outap, lo=lo, hi=hi: e.matmul(outap, lhsT=lhsT_ap, rhs=qrhs[:, lo:hi], start=True, stop=(tab_ap is None)),
                               reads=lreads + ["qT"], writes=sregs)
                            if tab_ap is not None:
                                op("pe", lambda e, outap=outap, lo=lo, hi=hi: e.matmul(outap, lhsT=identb[:], rhs=tab_ap[:, lo:hi], start=False, stop=True),
                                   reads=["identb", "wcast"], writes=sregs)

                    def pv_mm(obank, pt_ap, ptreg, v_ap, vreg, first, last):
                        for h in range(5):
                            op("pe", lambda e, h=h: e.matmul(bk(obank, h * 65, h * 65 + 65), lhsT=pt_ap[:, h * 128:(h + 1) * 128], rhs=v_ap, start=(first and h == 0), stop=last, skip_group_check=True),
                               reads=[ptreg, vreg], writes=["B%d" % obank])

                    def fin_den(obank, bi):
                        den = bk(obank, 0, 325).rearrange("p (h d) -> p h d", h=5)[:, :, 64]
                        op("dve", lambda e: e.tensor_scalar(out=rden[:, bi, :], in0=den, scalar1=1e-30, scalar2=None, op0=ALU.max), reads=["B%d" % obank], writes=["rden"])
                        op("dve", lambda e: e.reciprocal(out=rden[:, bi, :], in_=rden[:, bi, :]), reads=["rden"], writes=["rden"])
                        op("dve", lambda e: e.tensor_tensor(out=coef[:, bi, :], in0=rden[:, bi, :], in1=gk[:, :, bi], op=ALU.mult), reads=["rden", "gsig"], writes=["coef"])

                    cmax = min((16 * sb + 6) // 128, NCT - 1)
                    pendB = [None]
                    for c in range(cmax + 1):
                        e_ = j - 16 * c
                        ei = min(e_ // 2, 14)
                        cbt, cbreg = cb[(c) % 2], "cb%d" % (c % 2)
                        op("sp", lambda e, ei=ei, cbt=cbt: e.dma_start(out=cbt[:], in_=tabC_d[ei, kvh]), writes=[cbreg], dma_sem=cbreg)
                        sbk = 0
                        sregs = ["B0", "B1"]
                        scores(sbk, kcmpT[rows, c * 128:(c + 1) * 128], ["kcmpT"], None, sregs)
                        op("dve", lambda e, cbt=cbt: e.tensor_tensor(out=scs[:], in0=ps[:, 0:640], in1=cbt[:], op=ALU.add), reads=sregs + [cbreg], writes=["scs"])
                        ptb, ptreg = PTs[c % 3], "PTb%d" % (c % 3)
                        op("act", lambda e, ptb=ptb, c=c: e.activation(out=ptb[:], in_=scs[:], func=AF.Exp, bias=cmpvalid[:, c:c + 1]), reads=["scs", "cmpvalid"], writes=[ptreg])
                        def mk_c(ptb=ptb, ptreg=ptreg, c=c):
                            def f():
                                pv_mm(4, ptb, ptreg, vcmp[:, c, kvh, :], "vcmp", c == 0, c == cmax)
                                for h in range(5):
                                    ib = 2 + h // 3
                                    op("pe", lambda e, h=h, ib=ib: e.matmul(bk(ib, (h % 3) * NS, (h % 3) * NS + NS), lhsT=ptb[:, h * 128:(h + 1) * 128],
                                                                            rhs=ovs[:, c, :], start=(c == 0 and h % 3 == 0), stop=(c == cmax), skip_group_check=True),
                                       reads=[ptreg, "wcast"], writes=["B%d" % ib])
                            return f
                        if pendB[0] is not None:
                            pendB[0]()
                        pendB[0] = mk_c()
                    if pendB[0] is not None:
                        pendB[0]()
                        pendB[0] = None
                    chk("B_c")
                    fin_den(4, 0)
                    for h in range(5):
                        ib = 2 + h // 3
                        src = bk(ib, (h % 3) * NS, (h % 3) * NS + NS)
                        if h == 0:
                            op("dve", lambda e, src=src: e.tensor_scalar(out=imp[:, 0:NS], in0=src, scalar1=rden[:, 0, 0:1], scalar2=None, op0=ALU.mult),
                               reads=["B%d" % ib, "rden"], writes=["imp"])
                        else:
                            op("dve", lambda e, src=src, h=h: e.scalar_tensor_tensor(out=imp[:, 0:NS], in0=src, scalar=rden[:, 0, h:h + 1], in1=imp[:, 0:NS], op0=ALU.mult, op1=ALU.add),
                               reads=["B%d" % ib, "rden", "imp"], writes=["imp"])
                    op("dve", lambda e, t=sm_t: e.tensor_tensor(out=imp[:, 0:NS], in0=imp[:, 0:NS], in1=t[:], op=ALU.mult), reads=["imp", sm_reg], writes=["imp"])
                    op("dve", lambda e, t=sa_t: e.tensor_tensor(out=imp[:, 0:NS], in0=imp[:, 0:NS], in1=t[:], op=ALU.add), reads=["imp", sa_reg], writes=["imp"])
                    chk("B_i")
                    op("dve", lambda e: e.max(out=mx[:, 0:8], in_=imp[:]), reads=["imp"], writes=["mx"])
                    op("dve", lambda e: e.match_replace(out=impw[:], in_to_replace=mx[:, 0:8], in_values=imp[:], imm_value=-1.0e9), reads=["mx", "imp"], writes=["impw"])
                    op("dve", lambda e: e.max(out=mx[:, 8:16], in_=impw[:]), reads=["impw"], writes=["mx"])
                    op("dve", lambda e: e.tensor_scalar(out=selm[:, 0:NS], in0=imp[:, 0:NS], scalar1=mx[:, 15:16], scalar2=None, op0=ALU.is_ge), reads=["imp", "mx"], writes=["selm"])
                    for g in range(NSG):
                        op("pe", lambda e, g=g: e.matmul(ps[0:32, 7 * 512 + (g % 4) * 128: 7 * 512 + (g % 4 + 1) * 128], lhsT=selm[:, g * 32:(g + 1) * 32], rhs=identb[:], start=True, stop=True),
                           reads=["selm", "identb"], writes=["B7"])
                        if g % 4 == 3 or g == NSG - 1:
                            g0 = g - g % 4
                            ng = g - g0 + 1
                            op("dve", lambda e, g0=g0, ng=ng: e.tensor_copy(out=selT[:, g0:g0 + ng, :], in_=ps[0:32, 7 * 512: 7 * 512 + ng * 128].rearrange("p (g q) -> p g q", g=ng)),
                               reads=["B7"], writes=["selT"])
                    chk("B_t")
                    op("dve", lambda e: e.tensor_tensor(out=ob[:].rearrange("p (h d) -> p h d", h=5), in0=bk(4, 0, 325).rearrange("p (h d) -> p h d", h=5)[:, :, 0:64],
                                                        in1=coef[:, 0, :].unsqueeze(2).to_broadcast([128, 5, 64]), op=ALU.mult), reads=["B4", "coef"], writes=["ob"])

                    for kb in range(0, j + 1):
                        dl = min(j - kb, 13)
                        sbk, _ = sbufs.next()
                        sregs = ["B%d" % sbk, "B%d" % (sbk + 1)]
                        scores(sbk, ksT[rows, kb * 128:(kb + 1) * 128], ["ksT%d" % (kb // 2)], tabN[kvh][:, dl, :], sregs)
                        mreg = "B%d" % (6 if kb % 2 else 7)
                        mb = 6 if kb % 2 else 7
                        op("pe", lambda e, kb=kb, mb=mb: e.matmul(bk(mb, 0, 128), lhsT=EXs[:, (kb % 16) * 128:(kb % 16 + 1) * 128], rhs=selT[:, (2 * kb) // 32, :], start=True, stop=True),
                           reads=["selT", "wcast"], writes=[mreg])
                        ptb, ptreg = PTs[kb % 3], "PTb%d" % (kb % 3)
                        op("act", lambda e, sbk=sbk, ptb=ptb, kb=kb: e.activation(out=ptb[:], in_=ps[:, sbk * 512: sbk * 512 + 640], func=AF.Exp, bias=keyvalid[:, kb:kb + 1]),
                           reads=sregs + ["keyvalid"], writes=[ptreg])
                        p2, p2reg = PT2[kb % 2], "PT2b%d" % (kb % 2)
                        op("dve", lambda e, ptb=ptb, p2=p2, mb=mb: e.tensor_tensor(out=p2[:].rearrange("p (h q) -> p h q", h=5), in0=ptb[:].rearrange("p (h q) -> p h q", h=5),
                                                                                   in1=bk(mb, 0, 128).unsqueeze(1).to_broadcast([128, 5, 128]), op=ALU.mult),
                           reads=[ptreg, mreg], writes=[p2reg])
                        def mk_s(p2=p2, p2reg=p2reg, kb=kb):
                            def f():
                                pv_mm(5, p2, p2reg, vs[:, kb, kvh, :], "vs%d" % (kb // 2), kb == 0, kb == j)
                            return f
                        if pendB[0] is not None:
                            pendB[0]()
                        pendB[0] = mk_s()
                    if pendB[0] is not None:
                        pendB[0]()
                        pendB[0] = None
                    chk("B_s")
                    fin_den(5, 1)
                    op("dve", lambda e: e.tensor_tensor(out=ob2[:].rearrange("p (h d) -> p h d", h=5), in0=bk(5, 0, 325).rearrange("p (h d) -> p h d", h=5)[:, :, 0:64],
                                                        in1=coef[:, 1, :].unsqueeze(2).to_broadcast([128, 5, 64]), op=ALU.mult), reads=["B5", "coef"], writes=["ob2"])
                    op("dve", lambda e: e.tensor_tensor(out=ob[:], in0=ob[:], in1=ob2[:], op=ALU.add), reads=["ob", "ob2"], writes=["ob"])

                    kb0 = max(0, j - 4)
                    for kb in range(kb0, j + 1):
                        dl = j - kb
                        ti = 14 if dl == 4 else dl
                        wslot = kb % RW
                        sbk, _ = sbufs.next()
                        sregs = ["B%d" % sbk, "B%d" % (sbk + 1)]
                        scores(sbk, kwT[rows, wslot * 128:(wslot + 1) * 128], ["kw%d" % (wslot - wslot % 2)], tabN[kvh][:, ti, :], sregs)
                        ptb, ptreg = PTs[kb % 3], "PTb%d" % (kb % 3)
                        op("act", lambda e, sbk=sbk, ptb=ptb, kb=kb: e.activation(out=ptb[:], in_=ps[:, sbk * 512: sbk * 512 + 640], func=AF.Exp, bias=keyvalid[:, kb:kb + 1]),
                           reads=sregs + ["keyvalid"], writes=[ptreg])
                        def mk_w(ptb=ptb, ptreg=ptreg, kb=kb, wslot=wslot):
                            def f():
                                pv_mm(4, ptb, ptreg, vw[:, wslot, kvh, :], "vw%d" % (wslot - wslot % 2), kb == kb0, kb == j)
                            return f
                        if pendB[0] is not None:
                            pendB[0]()
                        pendB[0] = mk_w()
                    if pendB[0] is not None:
                        pendB[0]()
                        pendB[0] = None
                    chk("B_w")
                    fin_den(4, 2)
                    op("dve", lambda e: e.tensor_tensor(out=ob2[:].rearrange("p (h d) -> p h d", h=5), in0=bk(4, 0, 325).rearrange("p (h d) -> p h d", h=5)[:, :, 0:64],
                                                        in1=coef[:, 2, :].unsqueeze(2).to_broadcast([128, 5, 64]), op=ALU.mult), reads=["B4", "coef"], writes=["ob2"])
                    op("dve", lambda e, oBb=oBb, kvh=kvh: e.tensor_tensor(out=oBb[:, kvh * 320:(kvh + 1) * 320], in0=ob[:], in1=ob2[:], op=ALU.add),
                       reads=["ob", "ob2"], writes=[oreg])
                op("pool", lambda e, oBb=oBb, sb=sb: e.dma_start(out=o_scr[sb * 128:(sb + 1) * 128, 384:1024], in_=oBb[:]),
                   reads=[oreg], writes=["oscr%d" % sb], dma_sem=oreg)

        def pass_C():
          with contextlib.ExitStack() as st:
            wout = sbt(st, "wout", [128, 8, D], BF16)
            wrs = sbt(st, "wrs", [128, 8, 20], F32)
            brs = sbt(st, "brs", [128, 20], F32)
            hacc = [sbt(st, "hacc%d" % g, [128, D], F32) for g in range(G)]
            xn2T = [sbt(st, "xn2T%d" % g, [128, 8, 128], BF16) for g in range(G)]
            comb = [sbt(st, "comb%d" % g, [128, 16], F32) for g in range(G)]
            xc = [sbt(st, "xc%d" % i, [128, D], F32) for i in range(2)]
            oc = [sbt(st, "oc%d" % i, [128, D], BF16) for i in range(2)]
            oT = sbt(st, "oT", [128, 8, 128], BF16)
            xn2f = sbt(st, "xn2f", [128, D], F32)
            xn2b = sbt(st, "xn2b", [128, D], BF16)
            xn2Tf = sbt(st, "xn2Tf", [128, 8, 128], F32)
            ssq = sbt(st, "ssqc", [128, 2], F32)
            tmpn = sbt(st, "tmpnc", [128, 2], F32)
            junk = sbt(st, "junkc", [128, D], BF16)
            lg = sbt(st, "lg", [128, 20], F32)
            rt = sbt(st, "rt", [128, 64], F32)
            wst = [sbt(st, "wst%d" % i, [128, 4, 512], F32) for i in range(3)]
            wgb = [sbt(st, "wgb%d" % i, [128, 8, DE], BF16) for i in range(2)]
            wub = [sbt(st, "wub%d" % i, [128, 8, DE], BF16) for i in range(2)]
            wdb = [sbt(st, "wdb%d" % i, [128, 4, D], BF16) for i in range(2)]
            sgs = [sbt(st, "sg%d" % i, [128, DE], F32) for i in range(2)]
            hes = [sbt(st, "he%d" % i, [128, DE], BF16) for i in range(2)]
            heTs = [sbt(st, "heT%d" % i, [128, 4, 128], BF16) for i in range(2)]
            pcnt = [0]
            yout = [sbt(st, "yout%d" % i, [128, D], F32) for i in range(1)]

            for kc in range(8):
                load_cast(w_out[kc * 128:(kc + 1) * 128, :], wout[:, kc, :], 1024, eng="dve" if kc % 2 else "pool")
            op("sp", lambda e: e.dma_start(out=wrs[:], in_=wr_d.rearrange("(c p) n -> p c n", p=128)), writes=["wrs"], dma_sem="cst2", group=True)
            op("sp", lambda e: e.dma_start(out=brs[:], in_=br_d.rearrange("a n -> (a n)").partition_broadcast(128)), writes=["brs"], dma_sem="cst2", group=True)

            wst_rot = Rot("wst", wst)
            ngroups = 0 if stop == "C_init" else (NSB + G - 1) // G
            ybuf_i = [0]
            for grp in range(ngroups):
                blks = list(range(grp * G, min(NSB, (grp + 1) * G)))
                for gi, sb in enumerate(blks):
                    xct, xreg = xc[gi % 2], "xc%d" % (gi % 2)
                    oct_, ocreg = oc[gi % 2], "oc%d" % (gi % 2)
                    op("sp", lambda e, sb=sb, t=xct: e.dma_start(out=t[:], in_=xs[sb * 256: sb * 256 + 128, :]), writes=[xreg], dma_sem=xreg)
                    op("sp", lambda e, sb=sb, t=oct_: e.dma_start(out=t[:], in_=o_scr[sb * 128:(sb + 1) * 128, :]), reads=["oscr%d" % sb], writes=[ocreg], dma_sem=ocreg)
                    transpose_block(lambda k, t=oct_: t[:, k * 128:(k + 1) * 128], ocreg, oT[:], "oT", 8, 7, eng="act")
                    for n in range(2):
                        for kc in range(8):
                            op("pe", lambda e, n=n, kc=kc: e.matmul(bk(n), lhsT=oT[:, kc, :], rhs=wout[:, kc, n * 512:(n + 1) * 512], start=(kc == 0), stop=(kc == 7)),
                               reads=["oT", "wcast"], writes=["B%d" % n])
                    hreg = "hacc%d" % gi
                    op("dve", lambda e, gi=gi, t=xct: e.tensor_tensor(out=hacc[gi][:], in0=ps[:, 0:1024], in1=t[:], op=ALU.add), reads=["B0", "B1", xreg], writes=[hreg])
                    rmsnorm_block(hacc[gi][:], hreg, 1, xn2f[:], "xn2f", ssq[:, 0:1], tmpn[:, 0:1], junk[:], "c")
                    op("pool", lambda e: e.tensor_copy(out=xn2b[:], in_=xn2f[:]), reads=["xn2f"], writes=["xn2b"])
                    transpose_block(lambda k: xn2b[:, k * 128:(k + 1) * 128], "xn2b", xn2T[gi][:], "xn2T%d" % gi, 8, 7, eng="dve")
                    for half in range(2):
                        transpose_block(lambda k, half=half: xn2f[:, (half * 4 + k) * 128:(half * 4 + k + 1) * 128], "xn2f", xn2Tf[:, half * 4:(half + 1) * 4, :], "xn2Tf", 4, 6, fp32=True, eng="act")
                    for kc in range(8):
                        op("pe", lambda e, kc=kc: e.matmul(bk(5, 0, 20), lhsT=xn2Tf[:, kc, :], rhs=wrs[:, kc, :], start=(kc == 0), stop=(kc == 7)),
                           reads=["xn2Tf", "wrs"], writes=["B5"])
                    op("dve", lambda e: e.tensor_tensor(out=lg[:], in0=bk(5, 0, 20), in1=brs[:], op=ALU.add), reads=["B5", "brs"], writes=["lg"])
                    R = "rt"
                    def dv(fn, reads=(R, "lg"), writes=(R,)):
                        op("dve", fn, reads=list(reads), writes=list(writes))
                    dv(lambda e: e.tensor_reduce(out=rt[:, 0:1], in_=lg[:, 0:4], axis=mybir.AxisListType.X, op=ALU.max))
                    dv(lambda e: e.tensor_scalar(out=rt[:, 8:12], in0=lg[:, 0:4], scalar1=rt[:, 0:1], scalar2=None, op0=ALU.is_ge))
                    dv(lambda e: e.tensor_scalar(out=rt[:, 1:5], in0=lg[:, 0:4], scalar1=rt[:, 0:1], scalar2=None, op0=ALU.subtract))
                    dv(lambda e: e.memset(rt[:, 5:6], 0.0))
                    op("act", lambda e: e.activation(out=rt[:, 1:5], in_=rt[:, 1:5], func=AF.Exp, accum_out=rt[:, 5:6]), reads=[R], writes=[R])
                    dv(lambda e: e.reciprocal(out=rt[:, 6:7], in_=rt[:, 5:6]))
                    dv(lambda e: e.tensor_scalar(out=rt[:, 12:16], in0=lg[:, 4:8], scalar1=rt[:, 8:9], scalar2=None, op0=ALU.mult))
                    for g in range(1, 4):
                        dv(lambda e, g=g: e.scalar_tensor_tensor(out=rt[:, 12:16], in0=lg[:, 4 + 4 * g: 8 + 4 * g], scalar=rt[:, 8 + g: 9 + g], in1=rt[:, 12:16], op0=ALU.mult, op1=ALU.add))
                    dv(lambda e: e.tensor_reduce(out=rt[:, 16:17], in_=rt[:, 12:16], axis=mybir.AxisListType.X, op=ALU.max))
                    dv(lambda e: e.tensor_scalar(out=rt[:, 20:24], in0=rt[:, 12:16], scalar1=rt[:, 16:17], scalar2=None, op0=ALU.is_ge))
                    dv(lambda e: e.scalar_tensor_tensor(out=rt[:, 28:32], in0=rt[:, 20:24], scalar=-1.0e9, in1=rt[:, 12:16], op0=ALU.mult, op1=ALU.add))
                    dv(lambda e: e.tensor_reduce(out=rt[:, 17:18], in_=rt[:, 28:32], axis=mybir.AxisListType.X, op=ALU.max))
                    dv(lambda e: e.tensor_scalar(out=rt[:, 24:28], in0=rt[:, 28:32], scalar1=rt[:, 17:18], scalar2=None, op0=ALU.is_ge))
                    dv(lambda e: e.tensor_tensor(out=rt[:, 32:33], in0=rt[:, 17:18], in1=rt[:, 16:17], op=ALU.subtract))
                    op("act", lambda e: e.activation(out=rt[:, 33:34], in_=rt[:, 32:33], func=AF.Exp), reads=[R], writes=[R])
                    dv(lambda e: e.tensor_scalar(out=rt[:, 34:35], in0=rt[:, 33:34], scalar1=1.0, scalar2=None, op0=ALU.add))
                    dv(lambda e: e.reciprocal(out=rt[:, 35:36], in_=rt[:, 34:35]))
                    dv(lambda e: e.tensor_tensor(out=rt[:, 36:37], in0=rt[:, 33:34], in1=rt[:, 35:36], op=ALU.mult))
                    dv(lambda e: e.tensor_scalar(out=rt[:, 40:44], in0=rt[:, 20:24], scalar1=rt[:, 35:36], scalar2=None, op0=ALU.mult))
                    dv(lambda e: e.scalar_tensor_tensor(out=rt[:, 40:44], in0=rt[:, 24:28], scalar=rt[:, 36:37], in1=rt[:, 40:44], op0=ALU.mult, op1=ALU.add))
                    dv(lambda e: e.tensor_scalar(out=rt[:, 40:44], in0=rt[:, 40:44], scalar1=rt[:, 6:7], scalar2=None, op0=ALU.mult))
                    for g in range(4):
                        op("dve", lambda e, g=g, gi=gi: e.tensor_scalar(out=comb[gi][:, 4 * g: 4 * g + 4], in0=rt[:, 40:44], scalar1=rt[:, 8 + g: 9 + g], scalar2=None, op0=ALU.mult),
                           reads=[R], writes=["comb%d" % gi])

                pendC = [None]
                def load_expert(ex):
                    wslot = ex % 2
                    for (wd_, dstb, nm) in ((wg_d, wgb, "wg"), (wu_d, wub, "wu")):
                        for hf in range(2):
                            t, treg = wst_rot.next()
                            op("sp", lambda e, wd_=wd_, hf=hf, t=t, ex=ex: e.dma_start(out=t[:], in_=wd_[ex, hf * 512:(hf + 1) * 512, :].rearrange("(c p) n -> p c n", p=128)),
                               writes=[treg], dma_sem=treg)
                            op("act", lambda e, t=t, dstb=dstb, hf=hf, wslot=wslot: e.copy(out=dstb[wslot][:, hf * 4:(hf + 1) * 4, :], in_=t[:]),
                               reads=[treg], writes=["%s%d" % (nm, wslot)])
                    for hf in range(2):
                        t, treg = wst_rot.next()
                        op("sp", lambda e, hf=hf, t=t, ex=ex: e.dma_start(out=t[:].rearrange("p (c a) n -> p c (a n)", c=2),
                                                                         in_=wd_d[ex, hf * 256:(hf + 1) * 256, :].rearrange("(c p) n -> p c n", p=128)),
                           writes=[treg], dma_sem=treg)
                        op("pool", lambda e, t=t, hf=hf, wslot=wslot: e.tensor_copy(out=wdb[wslot][:, hf * 2:(hf + 1) * 2, :], in_=t[:].rearrange("p (c a) n -> p c (a n)", c=2)),
                           reads=[treg], writes=["wd%d" % wslot])

                load_expert(0)
                for ex in range(NE):
                    wslot = ex % 2
                    for gi, sb in enumerate(blks):
                        if gi == len(blks) // 2 and ex + 1 < NE:
                            load_expert(ex + 1)
                        gb = 0 if gi % 2 == 0 else 2
                        for (n, wb, nm) in ((0, wgb, "wg"), (1, wub, "wu")):
                            for kc in range(8):
                                op("pe", lambda e, n=n, wb=wb, kc=kc, gi=gi, gb=gb: e.matmul(bk(gb + n), lhsT=xn2T[gi][:, kc, :], rhs=wb[wslot][:, kc, :], start=(kc == 0), stop=(kc == 7)),
                                   reads=["xn2T%d" % gi, "%s%d" % (nm, wslot)], writes=["B%d" % (gb + n)])
                        pi = pcnt[0] % 2
                        pcnt[0] += 1
                        sg_t, he_t, heT_t = sgs[pi], hes[pi], heTs[pi]
                        op("act", lambda e, gb=gb, sg_t=sg_t: e.activation(out=sg_t[:], in_=bk(gb), func=AF.Silu), reads=["B%d" % gb], writes=["sg%d" % pi])
                        op("dve", lambda e, gb=gb, gi=gi, ex=ex, sg_t=sg_t, he_t=he_t: e.scalar_tensor_tensor(out=he_t[:], in0=bk(gb + 1), scalar=comb[gi][:, ex:ex + 1], in1=sg_t[:], op0=ALU.mult, op1=ALU.mult),
                           reads=["B%d" % (gb + 1), "comb%d" % gi, "sg%d" % pi], writes=["he%d" % pi])

                        def mk_c(pi=pi, he_t=he_t, heT_t=heT_t, gi=gi, wslot=wslot):
                            def f():
                                transpose_block(lambda k: he_t[:, k * 128:(k + 1) * 128], "he%d" % pi, heT_t[:], "heT%d" % pi, 4, 6, eng="act")
                                for n in range(2):
                                    for dc in range(4):
                                        op("pe", lambda e, n=n, dc=dc: e.matmul(bk(4 + n), lhsT=heT_t[:, dc, :], rhs=wdb[wslot][:, dc, n * 512:(n + 1) * 512], start=(dc == 0), stop=(dc == 3)),
                                           reads=["heT%d" % pi, "wd%d" % wslot], writes=["B%d" % (4 + n)])
                                op("dve", lambda e: e.tensor_tensor(out=hacc[gi][:], in0=hacc[gi][:], in1=ps[:, 4 * 512: 6 * 512], op=ALU.add),
                                   reads=["B4", "B5", "hacc%d" % gi], writes=["hacc%d" % gi])
                            return f
                        if pendC[0] is not None:
                            pendC[0]()
                        pendC[0] = mk_c()
                if pendC[0] is not None:
                    pendC[0]()
                    pendC[0] = None
                for gi, sb in enumerate(blks):
                    yb, yreg = yout[0], "yout0"
                    ybuf_i[0] += 1
                    rmsnorm_block(hacc[gi][:], "hacc%d" % gi, 2, yb[:], yreg, ssq[:, 1:2], tmpn[:, 1:2], junk[:], "f")
                    last_tok = op("pool", lambda e, yb=yb, sb=sb: e.dma_start(out=out_d[sb * 128:(sb + 1) * 128, :], in_=yb[:]), reads=[yreg], writes=["out%d" % sb], dma_sem=yreg)

        pass_A()
        if stop not in ("A_init", "A_1", "A"):
            S.barrier()
            pass_B()
            if stop not in ("B_init", "B_1", "B"):
                S.barrier()
                pass_C()
        finals = [("D", n, v[0]) for n, v in S.dma_sems.items() if n.startswith("yout") or n.startswith("oA") or n.startswith("oB")]
        S.emit(final_wait_tokens=finals)
    return nc


_PROG_CACHE = {}


def _prep_common(NB, inp):
    stt = _struct_tables(NB)
    relb = np.concatenate([np.asarray(inp["rel_bias"], np.float32), np.full((1, 16), NEG, np.float32)], axis=0)
    tN = relb[stt["idxN"]]
    tabN = np.stack([tN[..., 6 + 5 * k: 11 + 5 * k].transpose(0, 1, 3, 2).reshape(128, 15, 640) for k in range(2)], 0)
    tA = relb[stt["idxA"]][..., 0:6]
    tabA = tA.transpose(0, 1, 3, 2).reshape(128, 17, 768)
    tC = relb[stt["idxC"]]
    tabC = np.stack([tC[..., 6 + 5 * k: 11 + 5 * k].transpose(1, 0, 3, 2).reshape(15, 128, 640) for k in range(2)], 1)
    c = dict(
        w_in=np.ascontiguousarray(inp["w_in"][0]), w_out=np.ascontiguousarray(inp["w_out"][0]),
        gvec=np.ascontiguousarray(np.stack([inp["norm_mix"][0], inp["norm_ffn"][0], inp["norm_final"]], 0)),
        tabN=np.ascontiguousarray(tabN), tabA=np.ascontiguousarray(tabA), logm=stt["logm"], tabC=np.ascontiguousarray(tabC),
        ov=stt["ov"], EX=stt["EX"].reshape(32, 2048), E16=stt["E16"],
        posT=np.ascontiguousarray(np.stack([inp["cmp_pos_k"][0].T, inp["cmp_pos_v"][0].T], 0)),
        w1=np.ascontiguousarray(np.stack([inp["cmp_k_w1"][0], inp["cmp_v_w1"][0]], 0)),
        w2=np.ascontiguousarray(np.stack([inp["cmp_k_w2"][0], inp["cmp_v_w2"][0]], 0)),
        wr=np.ascontiguousarray(np.concatenate([inp["w_router_group"][0], inp["w_router_expert"][0].reshape(D, 16)], axis=1)),
        br=np.ascontiguousarray(np.concatenate([inp["b_router_group"][0], inp["b_router_expert"][0].reshape(16)])[None, :]),
        wg=np.ascontiguousarray(inp["w_gate"][0]), wu=np.ascontiguousarray(inp["w_up"][0]), wd=np.ascontiguousarray(inp["w_down"][0]),
    )
    return {k: np.asarray(v, np.float32) for k, v in c.items()}


def run(inp, debug=False, G=9, stop=None):
    x = np.asarray(inp["x"], np.float32)
    B, T, _ = x.shape
    NB = T // 128
    NBX = NB + 2
    NSB = NBX // 2
    key = (NB, debug, G)
    if key not in _PROG_CACHE:
        _PROG_CACHE[key] = build_program(NB, G=G, debug=debug, stop=stop)
    nc = _PROG_CACHE[key]
    common = _prep_common(NB, inp)
    ctabs = [_core_tables(NB, p) for p in range(2)]
    in_maps = []
    for c in range(2 * B):
        b, p = c // 2, c % 2
        xs = np.zeros((NBX * 128, D), np.float32)
        xs[p * 128: p * 128 + T] = x[b]
        m = dict(common)
        m["xs"] = xs
        m.update(ctabs[p])
        in_maps.append(m)
    res = run_bass_kernel_spmd(nc, in_maps, core_ids=list(range(2 * B)))
    out = np.zeros((B, T, D), np.float32)
    dbg = np.zeros((B, T, D), np.float32) if debug else None
    for c in range(2 * B):
        b, p = c // 2, c % 2
        o = np.asarray(res.results[c]["out"]).reshape(NSB, 128, D)
        for i in range(NSB):
            r = 2 * i - p
            if 0 <= r < NB:
                out[b, r * 128:(r + 1) * 128] = o[i]
                if debug:
                    dbg[b, r * 128:(r + 1) * 128] = np.asarray(res.results[c]["o_scr"]).astype(np.float32).reshape(NSB, 128, D)[i]
    if debug:
        return out, dbg
    return out


def kernel(**inputs):
    return run(inputs)
```

```python
import math
import contextlib
import numpy as np
import concourse.bass as bass
import concourse.mybir as mybir
from concourse.bass_utils import run_bass_kernel_spmd

F32 = mybir.dt.float32
BF16 = mybir.dt.bfloat16
ALU = mybir.AluOpType
AF = mybir.ActivationFunctionType

D = 1024
NIN = 2590
NEG = -30000.0
EPS = 1e-6
RA = 20
RW = 8
NE = 16
DE = 512


class Sched:
    ENG = ["pe", "dve", "act", "pool", "sp"]

    def __init__(self, nc):
        self.nc = nc
        self.ops = {e: [] for e in self.ENG}
        self.last_w = {}
        self.readers = {}
        self.known = {e: {} for e in self.ENG}
        self.dma_sems = {}
        self.group = set()
        self.pending = {e: [] for e in self.ENG}

    def barrier(self):
        toks = []
        for e in self.ENG:
            for i in range(len(self.ops[e]) - 1, -1, -1):
                if self.ops[e][i]["dma"] is None:
                    toks.append(("E", e, i))
                    break
        for n, v in self.dma_sems.items():
            toks.append(("D", n, v[0]))
        for e in self.ENG:
            self.pending[e] = list(toks)

    mute = False

    def op(self, eng, fn, reads=(), writes=(), dma_sem=None, group=False):
        if self.mute:
            return None
        ops = self.ops[eng]
        idx = len(ops)
        deps = list(self.pending[eng])
        self.pending[eng] = []
        for r in reads:
            t = self.last_w.get(r)
            if t is not None:
                deps.append(t)
            if len(r) == 2 and r[0] == "B" and r[1].isdigit():
                for t2 in self.readers.get(r, ()):
                    if t2[0] == "E" and t2[1] != eng:
                        deps.append(t2)
        for w in writes:
            t = self.last_w.get(w)
            if t is not None:
                deps.append(t)
            deps.extend(self.readers.get(w, ()))
        if dma_sem is not None:
            ent = self.dma_sems.setdefault(dma_sem, [0])
            ent[0] += 16
            mytok = ("D", dma_sem, ent[0])
            if group:
                self.group.add(dma_sem)
        else:
            mytok = ("E", eng, idx)
        waits = []
        for t in deps:
            if t[0] == "E":
                if t[1] == eng and (eng == "pe" or self.ops[eng][t[2]]["dma"] is not None):
                    continue
                key = ("E", t[1])
            else:
                key = ("D", t[1])
            if self.known[eng].get(key, -1) >= t[2]:
                continue
            self.known[eng][key] = t[2]
            waits.append(t)
        rec = _Recorder()
        fn(rec)
        ops.append(dict(call=rec.call, waits=waits, dma=mytok if dma_sem else None, signal=False))
        for t in waits:
            if t[0] == "E":
                self.ops[t[1]][t[2]]["signal"] = True
        for r in reads:
            self.readers.setdefault(r, []).append(mytok)
        for w in writes:
            self.last_w[w] = mytok
            self.readers[w] = []
        return mytok

    def emit(self, final_wait_tokens=()):
        nc = self.nc
        with contextlib.ExitStack() as st:
            esem = {e: st.enter_context(nc.semaphore("s_" + e)) for e in self.ENG}
            dsem = {n: st.enter_context(nc.semaphore("d_" + n)) for n in self.dma_sems}
            sigval = {}
            for e in self.ENG:
                c = 0
                for i, o in enumerate(self.ops[e]):
                    if o["signal"]:
                        c += 1
                        sigval[(e, i)] = c
            block = st.enter_context(nc.Block())

            def do_wait(eng, t):
                if t[0] == "E":
                    eng.wait_ge(esem[t[1]], sigval[(t[1], t[2])])
                else:
                    v = self.dma_sems[t[1]][0] if t[1] in self.group else t[2]
                    eng.wait_ge(dsem[t[1]], v)

            def run(e, eng):
                for i, o in enumerate(self.ops[e]):
                    for t in o["waits"]:
                        do_wait(eng, t)
                    cname, cargs, ckw = o["call"]
                    ins = getattr(eng, cname)(*cargs, **ckw)
                    if o["dma"] is not None:
                        ins.then_inc(dsem[o["dma"][1]], 16)
                    elif o["signal"]:
                        ins.then_inc(esem[e], 1)
                if e == "sp":
                    for t in final_wait_tokens:
                        do_wait(eng, t)

            @block.tensor
            def _(eng):
                run("pe", eng)

            @block.vector
            def _(eng):
                run("dve", eng)

            @block.scalar
            def _(eng):
                run("act", eng)

            @block.gpsimd
            def _(eng):
                run("pool", eng)

            @block.sync
            def _(eng):
                run("sp", eng)


class _Recorder:
    def __init__(self):
        self.call = None

    def __getattr__(self, name):
        def f(*a, **k):
            self.call = (name, a, k)
            return None
        return f


class Rot:
    def __init__(self, name, bufs):
        self.name, self.bufs, self.i = name, bufs, 0

    def next(self):
        k = self.i % len(self.bufs)
        self.i += 1
        return self.bufs[k], "%s%d" % (self.name, k)


def _bucket(dist):
    d = np.maximum(dist, 0)
    large = 16 + (np.log(np.maximum(d, 1).astype(np.float32) / np.float32(16)) / np.float32(math.log(128.0))
                  * np.float32(16)).astype(np.int32)
    large = np.minimum(large, 31)
    return np.where(d < 16, d, large).astype(np.int64)


def _struct_tables(NB):
    NBX = NB + 2
    TX = NBX * 128
    NS = TX // 64
    NCX = TX // 16 - 1
    NCT = (NCX + 127) // 128
    ki = np.arange(128)[:, None, None]
    qi = np.arange(128)[None, None, :]
    dl = np.arange(14)[None, :, None]
    dist = 128 * dl + qi - ki
    idxN = np.where(dist < 0, 32, _bucket(dist))
    distw = 128 * 4 + qi - ki
    idxW = np.where((distw < 0) | (distw >= 512), 32, _bucket(distw))
    idxN = np.concatenate([idxN, idxW], axis=1)
    dl = np.arange(17)[None, :, None]
    dist = 128 * dl + qi - ki
    mult = ((dist >= 0) & (dist <= 128)).astype(np.int64) + ((dist >= 0) & (dist <= 512) & (dist % 4 == 0)) \
        + ((dist >= 0) & (dist <= 2048) & (dist % 16 == 0))
    idxA = np.where(mult == 0, 32, _bucket(dist))
    logm = np.log(np.maximum(mult, 1).astype(np.float64)).astype(np.float32)
    ni = np.arange(128)[:, None, None]
    ee = (2 * np.arange(15))[None, :, None]
    dist = 128 * ee + qi - 16 * ni - 31
    idxC = np.where(dist < 0, 32, _bucket(dist))
    n = np.arange(NCT * 128)[:, None]
    s = np.arange(NS)[None, :]
    ov = np.clip(np.minimum(16 * n + 32, 64 * s + 64) - np.maximum(16 * n, 64 * s), 0, None).astype(np.float32) / 32.0
    ov = ov.reshape(NCT, 128, NS).transpose(1, 0, 2).copy()
    EX = np.zeros((32, 16, 128), np.float32)
    for m in range(16):
        for key in range(128):
            EX[(2 * m) % 32 + key // 64, m, key] = 1.0
    E16 = np.zeros((32, 272), np.float32)
    for k in range(16):
        E16[k, 128 + k] = 1.0
    return dict(idxN=idxN, idxA=idxA, logm=logm, idxC=idxC, ov=ov, EX=EX, E16=E16)


def _core_tables(NB, p):
    NBX = NB + 2
    NSB = NBX // 2
    TX = NBX * 128
    NS = TX // 64
    NCX = TX // 16 - 1
    NCT = (NCX + 127) // 128
    n_cmp = (NB * 128 - 32) // 16 + 1
    keyvalid = np.full((128, NBX), NEG, np.float32)
    keyvalid[:, p:p + NB] = 0.0
    nn = np.arange(NCT * 128)
    cv = np.where((nn >= 8 * p) & (nn <= 8 * p + n_cmp - 1), 0.0, NEG).astype(np.float32)
    cmpvalid = cv.reshape(NCT, 128).T.copy()
    s0 = 2 * p
    s_last = s0 + NB * 2 - 1
    selmul = np.zeros((NSB, 128, NS), np.float32)
    seladd = np.zeros((NSB, 128, NS), np.float32)
    blk = np.arange(NS)[None, :]
    for sb in range(NSB):
        t = 256 * sb + np.arange(128)
        cur = (t // 64)[:, None]
        valid = (blk >= s0) & (blk <= cur) & (blk <= s_last)
        mul = valid.astype(np.float32)
        add = np.where(blk > cur, -1.0, 0.0)
        add = np.where((blk < s0) | (blk > s_last), -2.0, add)
        f3 = valid & (blk == s0)
        f2 = valid & (blk == cur - 1)
        f1 = valid & (blk == cur)
        for f, v in ((f3, 3.0e4), (f2, 2.0e4), (f1, 1.0e4)):
            add = np.where(f, v, add)
            mul = np.where(f, 0.0, mul)
        selmul[sb] = mul
        seladd[sb] = add
    return dict(keyvalid=keyvalid, cmpvalid=cmpvalid, selmul=selmul, seladd=seladd)


class _Stop(Exception):
    pass


def build_program(NB, G=9, debug=False, stop=None):
    NBX = NB + 2
    NSB = NBX // 2
    TX = NBX * 128
    NS = TX // 64
    NSG = (NS + 31) // 32
    NCX = TX // 16 - 1
    NCT = (NCX + 127) // 128
    assert 3 * NS <= 512

    nc = bass.Bass("TRN2", target_bir_lowering=False)

    def din(name, shape, dt=F32):
        return nc.dram_tensor(name, list(shape), dt, kind="ExternalInput").ap()

    xs = din("xs", [TX, D])
    w_in = din("w_in", [D, NIN])
    w_out = din("w_out", [D, D])
    gvec = din("gvec", [3, D])
    tabN_d = din("tabN", [2, 128, 15, 640])
    tabA_d = din("tabA", [128, 17, 768])
    logm_d = din("logm", [128, 17, 128])
    tabC_d = din("tabC", [15, 2, 128, 640])
    ov_d = din("ov", [128, NCT, NS])
    EX_d = din("EX", [32, 16 * 128])
    E16_d = din("E16", [32, 272])
    posT_d = din("posT", [2, 64, 32])
    w1_d = din("w1", [2, 2048, 256])
    w2_d = din("w2", [2, 256, 64])
    keyvalid_d = din("keyvalid", [128, NBX])
    cmpvalid_d = din("cmpvalid", [128, NCT])
    selmul_d = din("selmul", [NSB, 128, NS])
    seladd_d = din("seladd", [NSB, 128, NS])
    wr_d = din("wr", [D, 20])
    br_d = din("br", [1, 20])
    wg_d = din("wg", [NE, D, DE])
    wu_d = din("wu", [NE, D, DE])
    wd_d = din("wd", [NE, DE, D])
    out_d = nc.dram_tensor("out", [NSB * 128, D], F32, kind="ExternalOutput").ap()
    o_scr = nc.dram_tensor("o_scr", [NSB * 128, D], BF16, kind="ExternalOutput" if debug else "Internal").ap()

    S = Sched(nc)
    op = S.op
    with contextlib.ExitStack() as top:
        _uid = [0]

        def sbt(st, name, shape, dt):
            _uid[0] += 1
            return st.enter_context(nc.sbuf_tensor("sb%d_%s" % (_uid[0], name), list(shape), dt))

        ps = top.enter_context(nc.psum_tensor("ps", [128, 4096], F32))

        def bk(b, lo=0, hi=512):
            return ps[:, b * 512 + lo: b * 512 + hi]

        def bkb(b):
            return ps[:, b * 512:(b + 1) * 512].bitcast(BF16)

        identf = sbt(top, "identf", [128, 128], F32)
        identb = sbt(top, "identb", [128, 128], BF16)
        gbc = sbt(top, "gbc", [128, 3, D], F32)
        epsc = sbt(top, "epsc", [128, 1], F32)
        keyvalid = sbt(top, "keyvalid", [128, NBX], F32)
        cmpvalid = sbt(top, "cmpvalid", [128, NCT], F32)
        kcmpT = sbt(top, "kcmpT", [128, NCT * 128], BF16)
        vcmp = sbt(top, "vcmp", [128, NCT, 2, 65], BF16)
        stg = sbt(top, "stg", [128, 2048], F32)

        op("pool", lambda e: e.memset(identf[:], 1.0), writes=["identf"])
        op("pool", lambda e: e.affine_select(out=identf[:], in_=identf[:], pattern=[[1, 128]], compare_op=ALU.is_equal,
                                             fill=0.0, base=0, channel_multiplier=-1), reads=["identf"], writes=["identf"])
        op("pool", lambda e: e.tensor_copy(out=identb[:], in_=identf[:]), reads=["identf"], writes=["identb"])
        op("pool", lambda e: e.memset(epsc[:], EPS), writes=["epsc"])
        op("pool", lambda e: e.memset(kcmpT[:], 0.0), writes=["kcmpT"])
        op("pool", lambda e: e.memset(vcmp[:], 0.0), writes=["vcmp"])
        op("pool", lambda e: e.memset(vcmp[:, :, :, 64:65], 1.0), reads=["vcmp"], writes=["vcmp"])
        op("sp", lambda e: e.dma_start(out=gbc[:].rearrange("p a d -> p (a d)"),
                                       in_=gvec.rearrange("a d -> (a d)").partition_broadcast(128)), writes=["gbc"], dma_sem="cst", group=True)
        op("sp", lambda e: e.dma_start(out=keyvalid[:], in_=keyvalid_d), writes=["keyvalid"], dma_sem="cst", group=True)
        op("sp", lambda e: e.dma_start(out=cmpvalid[:], in_=cmpvalid_d), writes=["cmpvalid"], dma_sem="cst", group=True)

        def load_cast(src_ap, dst_ap, n, eng="dve", stg_view=None):
            sv = stg[:, 0:n] if stg_view is None else stg_view
            op("sp", lambda e, s=src_ap, v=sv: e.dma_start(out=v, in_=s), writes=["stg"], dma_sem="stg")
            op(eng, lambda e, v=sv, d=dst_ap: e.tensor_copy(out=d, in_=v), reads=["stg"], writes=["wcast"])

        def rmsnorm_block(xin_ap, xin_reg, gi, out_ap, out_reg, ssq_ap, tmp_ap, junk_ap, tag):
            op("dve", lambda e: e.memset(ssq_ap, 0.0), writes=["st_" + tag])
            op("act", lambda e: e.activation(out=junk_ap, in_=xin_ap, func=AF.Square, accum_out=ssq_ap),
               reads=[xin_reg, "st_" + tag], writes=["junk_" + tag, "st_" + tag])
            op("act", lambda e: e.activation(out=tmp_ap, in_=ssq_ap, func=AF.Sqrt, bias=epsc[:], scale=1.0 / D),
               reads=["st_" + tag, "epsc"], writes=["st_" + tag])
            op("dve", lambda e: e.reciprocal(out=tmp_ap, in_=tmp_ap), reads=["st_" + tag], writes=["st_" + tag])
            op("dve", lambda e: e.scalar_tensor_tensor(out=out_ap, in0=xin_ap, scalar=tmp_ap, in1=gbc[:, gi, :],
                                                       op0=ALU.mult, op1=ALU.mult),
               reads=[xin_reg, "st_" + tag, "gbc"], writes=[out_reg])

        tpbank = [7]

        def transpose_block(src_ap_fn, src_reg, dst_ap, dst_reg, nch, bank, fp32=False, eng="act"):
            breg = "B%d" % bank
            if fp32:
                for k in range(nch):
                    op("pe", lambda e, k=k: e.transpose(out=bk(bank, k * 128, (k + 1) * 128), in_=src_ap_fn(k), identity=identf[:]),
                       reads=[src_reg, "identf"], writes=[breg])
                src = bk(bank, 0, nch * 128).rearrange("p (k t) -> p k t", k=nch)
            else:
                for k in range(nch):
                    op("pe", lambda e, k=k: e.transpose(out=bkb(bank)[:, k * 128:(k + 1) * 128], in_=src_ap_fn(k), identity=identb[:]),
                       reads=[src_reg, "identb"], writes=[breg])
                src = bkb(bank)[:, 0:nch * 128].rearrange("p (k t) -> p k t", k=nch)
            if eng == "act":
                op("act", lambda e: e.copy(out=dst_ap, in_=src), reads=[breg], writes=[dst_reg])
            else:
                op("dve", lambda e: e.tensor_copy(out=dst_ap, in_=src), reads=[breg], writes=[dst_reg])

        evac_flip = [0]

        def evac(dst_ap, src_ap, reads, writes, scale=None, eng=None):
            if eng is None:
                evac_flip[0] ^= 1
                use_act = bool(evac_flip[0])
            else:
                use_act = (eng == "act")
            if use_act:
                if scale is None:
                    op("act", lambda e: e.copy(out=dst_ap, in_=src_ap), reads=reads, writes=writes)
                else:
                    op("act", lambda e: e.mul(out=dst_ap, in_=src_ap, mul=scale), reads=reads, writes=writes)
            else:
                if scale is None:
                    op("dve", lambda e: e.tensor_copy(out=dst_ap, in_=src_ap), reads=reads, writes=writes)
                else:
                    op("dve", lambda e: e.tensor_scalar(out=dst_ap, in0=src_ap, scalar1=scale, scalar2=None, op0=ALU.mult),
                       reads=reads, writes=writes)

        def load_norm_transpose(st_bufs, sb, nblk, gi):
            (xt, xn, xnT, ssq, tmp, junk) = st_bufs["cur"]()
            xt_t, xt_reg = xt
            xnT_t, xnT_reg = xnT
            op("sp", lambda e: e.dma_start(out=xt_t[:, 0:nblk, :], in_=xs[sb * 256: sb * 256 + nblk * 128, :].rearrange("(b p) d -> p b d", p=128)),
               writes=[xt_reg], dma_sem=xt_reg)
            for b in range(nblk):
                rmsnorm_block(xt_t[:, b, :], xt_reg, gi, xn[:, b, :], "xn", ssq[:, b:b + 1], tmp[:, b:b + 1], junk[:], "n%d" % b)
                transpose_block(lambda k, b=b: xn[:, b, k * 128:(k + 1) * 128], "xn", xnT_t[:, :, b * 128:(b + 1) * 128], xnT_reg, 8, 7,
                                eng="act" if b == 0 else "dve")
            return xt_t, xt_reg, xnT_t, xnT_reg

        pjbanks = Rot("B", [0, 1, 2, 3])

        def proj_fm(w_ap_fn, xnT_t, xnT_reg, ntok, dst_ap, dst_reg, wreg, scale=None):
            b = pjbanks.bufs[pjbanks.i % 4]
            pjbanks.i += 1
            breg = "B%d" % b
            for kc in range(8):
                op("pe", lambda e, kc=kc: e.matmul(bk(b, 0, ntok), lhsT=w_ap_fn(kc), rhs=xnT_t[:, kc, 0:ntok], start=(kc == 0), stop=(kc == 7)),
                   reads=[wreg, xnT_reg], writes=[breg])
            evac(dst_ap, bk(b, 0, ntok), [breg], [dst_reg], scale)

        def proj_tm(xnT_t, xnT_reg, blk, w_ap_fn, ncols, wreg):
            b = pjbanks.bufs[pjbanks.i % 4]
            pjbanks.i += 1
            breg = "B%d" % b
            for kc in range(8):
                op("pe", lambda e, kc=kc: e.matmul(bk(b, 0, ncols), lhsT=xnT_t[:, kc, blk * 128:(blk + 1) * 128], rhs=w_ap_fn(kc),
                                                   start=(kc == 0), stop=(kc == 7)),
                   reads=[wreg, xnT_reg], writes=[breg])
            return bk(b, 0, ncols), breg

        def make_xbufs(st):
            xts = [sbt(st, "xt%d" % i, [128, 2, D], F32) for i in range(2)]
            xnTs = [sbt(st, "xnT%d" % i, [128, 8, 256], BF16) for i in range(2)]
            xn = sbt(st, "xn", [128, 2, D], BF16)
            ssq = sbt(st, "ssq", [128, 2], F32)
            tmp = sbt(st, "tmpn", [128, 2], F32)
            junk = sbt(st, "junk", [128, D], BF16)
            cnt = [0]

            def cur():
                k = cnt[0] % 2
                cnt[0] += 1
                return ((xts[k], "xt%d" % k), xn, (xnTs[k], "xnT%d" % k), ssq, tmp, junk)
            return {"cur": cur}

        def chk(name):
            if stop == name:
                S.mute = True

        def pass_A():
          with contextlib.ExitStack() as st:
            winA = sbt(st, "winA", [128, 8, 1408], BF16)
            w1s = sbt(st, "w1s", [128, 2, 32, 256], BF16)
            w2s = sbt(st, "w2s", [128, 2, 2, 128], BF16)
            posT = sbt(st, "posT", [128, 2, 32], BF16)
            c1 = sbt(st, "c1", [128, 4], F32)
            tabA = sbt(st, "tabA", [128, 17, 768], BF16)
            E16 = sbt(st, "E16", [32, 272], BF16)
            kAT = sbt(st, "kAT", [128, 3, RA * 128], BF16)
            vA = sbt(st, "vA", [128, RA, 6, 65], BF16)
            kct = [sbt(st, "kct%d" % i, [128, 2, 272], BF16) for i in range(2)]
            qAT = sbt(st, "qAT", [128, 3, 128], BF16)
            PTs = [sbt(st, "PTa%d" % i, [128, 768], BF16) for i in range(3)]
            kgath = sbt(st, "kgath", [128, 2, 32, 16], BF16)
            h1u = sbt(st, "h1u", [128, 128], F32)
            h1a = sbt(st, "h1a", [128, 128], F32)
            h1b = sbt(st, "h1b", [128, 128], F32)
            h1g = sbt(st, "h1g", [128, 160], BF16)
            vnew = sbt(st, "vnew", [32, 2, 64], BF16)
            rdenA = sbt(st, "rdenA", [128, 6], F32)
            oA = [sbt(st, "oA%d" % i, [128, 384], BF16) for i in range(2)]
            xb = make_xbufs(st)

            for kc in range(8):
                load_cast(w_in[kc * 128:(kc + 1) * 128, 0:1152], winA[:, kc, 0:1152], 1152, eng="dve")
                load_cast(w_in[kc * 128:(kc + 1) * 128, 1792:2048], winA[:, kc, 1152:1408], 256, eng="pool")
            for kv in range(2):
                for lc in range(4):
                    for half in range(2):
                        sv = stg[half * 64:(half + 1) * 64, :]
                        src = w1_d[kv, lc * 512:(lc + 1) * 512, :].rearrange("(l d) h -> d l h", d=64)
                        dst = w1s[half * 64:(half + 1) * 64, kv, lc * 8:(lc + 1) * 8, :]
                        op("sp", lambda e, s=src, v=sv: e.dma_start(out=v.rearrange("p (l h) -> p l h", l=8), in_=s), writes=["stg"], dma_sem="stg")
                        op("dve" if half else "pool", lambda e, v=sv, d=dst: e.tensor_copy(out=d, in_=v.rearrange("p (l h) -> p l h", l=8)),
                           reads=["stg"], writes=["wcast"])
                for dup in range(2):
                    load_cast(w2_d[kv].rearrange("(c p) d -> p c d", p=128), w2s[:, kv, :, dup * 64:(dup + 1) * 64], 128, stg_view=stg[:, 0:128].rearrange("p (c d) -> p c d", c=2))
                for half in range(2):
                    load_cast(posT_d[kv], posT[half * 64:(half + 1) * 64, kv, :], 32, stg_view=stg[half * 64:(half + 1) * 64, 0:32])
            load_cast(E16_d, E16[:], 272, stg_view=stg[0:32, 0:272])
            for dl in range(17):
                op("sp", lambda e, dl=dl: e.dma_start(out=stg[:, 0:768], in_=tabA_d[:, dl, :]), writes=["stg"], dma_sem="stg")
                op("sp", lambda e, dl=dl: e.dma_start(out=stg[:, 768:896], in_=logm_d[:, dl, :]), writes=["stg2"], dma_sem="stg2")
                op("dve", lambda e, dl=dl: e.tensor_tensor(out=tabA[:, dl, :].rearrange("p (h q) -> p h q", h=6),
                                                           in0=stg[:, 0:768].rearrange("p (h q) -> p h q", h=6),
                                                           in1=stg[:, 768:896].unsqueeze(1).to_broadcast([128, 6, 128]), op=ALU.add),
                   reads=["stg", "stg2"], writes=["wcast"])
            op("pool", lambda e: e.memset(vA[:, :, :, 64:65], 1.0), writes=["vA%d" % s_ for s_ in range(0, RA, 2)])
            op("pool", lambda e: e.memset(h1u[:], 0.0), writes=["h1u"])
            op("pool", lambda e: e.memset(h1g[:], 0.0), writes=["h1g"])
            op("pool", lambda e: e.memset(kct[0][:], 0.0), writes=["kct0"])
            op("pool", lambda e: e.memset(kct[1][:], 0.0), writes=["kct1"])
            for kv in range(2):
                for hc in range(2):
                    col = kv * 2 + hc
                    for l in range(32):
                        op("pe", lambda e, kv=kv, hc=hc, l=l, col=col: e.matmul(bk(6, col, col + 1), lhsT=w1s[0:64, kv, l, hc * 128:(hc + 1) * 128],
                                                                                 rhs=posT[0:64, kv, l:l + 1], start=(l == 0), stop=(l == 31)),
                           reads=["wcast"], writes=["B6"])
            op("dve", lambda e: e.tensor_copy(out=c1[:], in_=bk(6, 0, 4)), reads=["B6"], writes=["c1"])

            sbufs = Rot("SA", [0, 2])
            for sb in range(0 if stop == "A_init" else NSB):
                if sb == 1 and stop == "A_1":
                    break
                j = 2 * sb
                xt_t, xt_reg, xnT_t, xnT_reg = load_norm_transpose(xb, sb, 2, 0)
                chk("A_1a")
                slot0 = j % RA
                for pr in range(3):
                    proj_fm(lambda kc, pr=pr: winA[:, kc, 384 + pr * 128: 384 + (pr + 1) * 128], xnT_t, xnT_reg, 256,
                            kAT[:, pr, slot0 * 128: slot0 * 128 + 256], "kA%d" % slot0, "wcast")
                kc_t = kct[sb % 2]
                kc_reg = "kct%d" % (sb % 2)
                kp_t = kct[(sb + 1) % 2]
                kp_reg = "kct%d" % ((sb + 1) % 2)
                if sb > 0:
                    op("pool", lambda e, a=kc_t, b=kp_t: e.tensor_copy(out=a[:, :, 0:16], in_=b[:, :, 256:272]), reads=[kp_reg], writes=[kc_reg])
                for kv in range(2):
                    proj_fm(lambda kc, kv=kv: winA[:, kc, 1152 + kv * 128: 1152 + (kv + 1) * 128], xnT_t, xnT_reg, 256,
                            kc_t[:, kv, 16:272], kc_reg, "wcast")
                for pr in range(3):
                    proj_fm(lambda kc, pr=pr: winA[:, kc, pr * 128:(pr + 1) * 128], xnT_t, xnT_reg, 128, qAT[:, pr, :], "qAT", "wcast", scale=0.125)
                for b in range(2):
                    pv, breg = proj_tm(xnT_t, xnT_reg, b, lambda kc: winA[:, kc, 768:1152], 384, "wcast")
                    evac(vA[:, slot0 + b, :, 0:64], pv.rearrange("p (h d) -> p h d", h=6), [breg], ["vA%d" % slot0])
                chk("A_1b")
                i0 = 0
                cnt = 16
                nfirst = 16 * sb - 1
                assert nfirst + cnt <= NCX
                if cnt > 0:
                    for kv in range(2):
                        op("dve", lambda e, kv=kv: e.tensor_copy(out=kgath[:, kv, :, :], in_=kc_t[:, kv, :].rearrange("p (n l) -> p l n", l=16)[:, :, 0:16]) if False else
                           e.tensor_copy(out=kgath[:, kv, 0:16, :], in_=kc_t[:, kv, 0:256].rearrange("p (n l) -> p l n", l=16)),
                           reads=[kc_reg], writes=["kgath"])
                        op("dve", lambda e, kv=kv: e.tensor_copy(out=kgath[:, kv, 16:32, :], in_=kc_t[:, kv, 16:272].rearrange("p (n l) -> p l n", l=16)),
                           reads=[kc_reg], writes=["kgath"])
                    chk("A_cg")
                    for kv in range(2):
                        for kvh in range(2):
                            rows = slice(kvh * 64, (kvh + 1) * 64)
                            for hc in range(2):
                                grp = (kv * 2 + kvh) * 2 + hc
                                for l in range(32):
                                    hb = 6 if kvh == 0 else 4
                                    hoff = 0 if kvh == 0 else 256
                                    op("pe", lambda e, kv=kv, rows=rows, hc=hc, grp=grp, l=l, hb=hb, hoff=hoff: e.matmul(
                                        bk(hb, hoff + grp * 16, hoff + grp * 16 + cnt), lhsT=w1s[rows, kv, l, hc * 128:(hc + 1) * 128],
                                        rhs=kgath[rows, kv, l, :], start=(l == 0), stop=(l == 31)),
                                       reads=["wcast", "kgath"], writes=["B%d" % hb])
                    chk("A_c0")
                    for grp in range(8):
                        kv, hc = grp // 4, grp % 2
                        kvh_ = (grp // 2) % 2
                        hb = 6 if kvh_ == 0 else 4
                        hoff = 0 if kvh_ == 0 else 256
                        op("dve", lambda e, grp=grp, kv=kv, hc=hc, hb=hb, hoff=hoff: e.tensor_scalar(out=h1u[:, grp * 16: grp * 16 + cnt], in0=bk(hb, hoff + grp * 16, hoff + grp * 16 + cnt),
                                                                                    scalar1=c1[:, kv * 2 + hc: kv * 2 + hc + 1], scalar2=None, op0=ALU.add),
                           reads=["B%d" % hb, "c1"], writes=["h1u"])
                    chk("A_c1")
                    op("dve", lambda e: e.tensor_tensor(out=h1a[:], in0=h1u[:], in1=h1u[:], op=ALU.mult), reads=["h1u"], writes=["h1a"])
                    op("dve", lambda e: e.tensor_scalar(out=h1a[:], in0=h1a[:], scalar1=0.044715, scalar2=1.0, op0=ALU.mult, op1=ALU.add), reads=["h1a"], writes=["h1a"])
                    op("dve", lambda e: e.tensor_tensor(out=h1a[:], in0=h1a[:], in1=h1u[:], op=ALU.mult), reads=["h1a", "h1u"], writes=["h1a"])
                    op("act", lambda e: e.activation(out=h1b[:], in_=h1a[:], func=AF.Exp, scale=-1.5957691216057308), reads=["h1a"], writes=["h1b"])
                    op("dve", lambda e: e.tensor_scalar(out=h1b[:], in0=h1b[:], scalar1=1.0, scalar2=None, op0=ALU.add), reads=["h1b"], writes=["h1b"])
                    op("dve", lambda e: e.reciprocal(out=h1b[:], in_=h1b[:]), reads=["h1b"], writes=["h1b"])
                    op("dve", lambda e: e.tensor_tensor(out=h1g[:, 0:128], in0=h1u[:], in1=h1b[:], op=ALU.mult), reads=["h1u", "h1b"], writes=["h1g"])
                    chk("A_c2")
                    for kvh in range(2):
                        for hc in range(2):
                            grp = (1 * 2 + kvh) * 2 + hc
                            op("pe", lambda e, kvh=kvh, hc=hc, grp=grp: e.matmul(ps[0:32, 5 * 512 + 64 + kvh * 64: 5 * 512 + 64 + (kvh + 1) * 64],
                                                                                 lhsT=h1g[:, grp * 16: grp * 16 + 32], rhs=w2s[:, 1, hc, 0:64],
                                                                                 start=(hc == 0), stop=(hc == 1)),
                               reads=["wcast", "h1g"], writes=["B5"])
                    op("dve", lambda e: e.tensor_copy(out=vnew[:, :, :], in_=ps[0:32, 5 * 512 + 64: 5 * 512 + 192].rearrange("p (k d) -> p k d", k=2)),
                       reads=["B5"], writes=["vnew"])
                    chk("A_c3")
                    c_lo = nfirst // 128
                    c_hi = (nfirst + cnt - 1) // 128
                    for c in range(max(c_lo, 0), c_hi + 1):
                        base = nfirst - 128 * c
                        op("pe", lambda e, base=base: e.matmul(bk(4, 0, 128), lhsT=E16[:, 128 - base: 256 - base],
                                                               rhs=vnew[:, :, :].rearrange("p k d -> p (k d)"), start=True, stop=True),
                           reads=["vnew", "wcast"], writes=["B4"])
                        op("dve", lambda e, c=c: e.tensor_tensor(out=vcmp[:, c, :, 0:64], in0=vcmp[:, c, :, 0:64],
                                                                 in1=bk(4, 0, 128).rearrange("p (k d) -> p k d", k=2), op=ALU.add),
                           reads=["B4", "vcmp"], writes=["vcmp"])
                    chk("A_c4")
                    for kvh in range(2):
                        for hc in range(2):
                            grp = (0 * 2 + kvh) * 2 + hc
                            op("pe", lambda e, kvh=kvh, hc=hc, grp=grp: e.matmul(bk(4, 448 + kvh * 16, 448 + kvh * 16 + cnt),
                                                                                 lhsT=w2s[:, 0, hc, :],
                                                                                 rhs=h1g[:, grp * 16: grp * 16 + cnt], start=(hc == 0), stop=(hc == 1)),
                               reads=["wcast", "h1g"], writes=["B4"])
                    for kvh in range(2):
                        rows = slice(kvh * 64, (kvh + 1) * 64)
                        d0 = 1 if nfirst < 0 else 0
                        op("dve", lambda e, kvh=kvh, rows=rows, d0=d0: e.tensor_copy(out=kcmpT[rows, nfirst + d0: nfirst + cnt],
                                                                                     in_=ps[rows, 4 * 512 + 448 + kvh * 16 + d0: 4 * 512 + 448 + kvh * 16 + cnt]),
                           reads=["B4"], writes=["kcmpT"])

                chk("A_1c")
                kb0 = max(0, j - 16)
                pendA = [None]
                for kb in range(kb0, j + 1):
                    dl = j - kb
                    slot = kb % RA
                    sreg_slot = "kA%d" % (slot - slot % 2)
                    vreg_slot = "vA%d" % (slot - slot % 2)
                    sbk, _ = sbufs.next()
                    sregs = ["B%d" % sbk, "B%d" % (sbk + 1)]
                    for h in range(6):
                        rows = slice((h % 2) * 64, (h % 2) * 64 + 64)
                        outap = ps[:, (sbk + h % 2) * 512 + (h // 2) * 128: (sbk + h % 2) * 512 + (h // 2 + 1) * 128]
                        op("pe", lambda e, h=h, rows=rows, outap=outap, slot=slot: e.matmul(outap, lhsT=kAT[rows, h // 2, slot * 128:(slot + 1) * 128],
                                                                                             rhs=qAT[rows, h // 2, :], start=True, stop=False),
                           reads=[sreg_slot, "qAT"], writes=sregs)
                        op("pe", lambda e, h=h, outap=outap, dl=dl: e.matmul(outap, lhsT=identb[:], rhs=tabA[:, dl, h * 128:(h + 1) * 128], start=False, stop=True),
                           reads=["identb", "wcast"], writes=sregs)
                    PT, ptreg = None, None
                    ptb = PTs[(kb - kb0) % 3]
                    ptreg = "PTa%d" % ((kb - kb0) % 3)
                    op("act", lambda e, sbk=sbk, ptb=ptb, kb=kb: e.activation(out=ptb[:].rearrange("p (a c) -> p a c", a=2), in_=ps[:, sbk * 512: sbk * 512 + 1024].rearrange("p (a c) -> p a c", a=2)[:, :, 0:384], func=AF.Exp,
                                                                               bias=keyvalid[:, kb:kb + 1]),
                       reads=sregs + ["keyvalid"], writes=[ptreg])
                    def mk_pv(ptb=ptb, ptreg=ptreg, slot=slot, kb=kb, vreg_slot=vreg_slot):
                        def f():
                            for h in range(6):
                                op("pe", lambda e, h=h: e.matmul(bk(5, h * 65, h * 65 + 65), lhsT=ptb[:, (h % 2) * 384 + (h // 2) * 128: (h % 2) * 384 + (h // 2 + 1) * 128],
                                                                 rhs=vA[:, slot, h, :], start=(kb == kb0 and h == 0), stop=(kb == j), skip_group_check=True),
                                   reads=[ptreg, vreg_slot], writes=["B5"])
                        return f
                    if pendA[0] is not None:
                        pendA[0]()
                    pendA[0] = mk_pv()
                if pendA[0] is not None:
                    pendA[0]()
                    pendA[0] = None
                op("dve", lambda e: e.tensor_scalar(out=rdenA[:], in0=bk(5, 0, 390).rearrange("p (h d) -> p h d", h=6)[:, :, 64], scalar1=1e-30, scalar2=None, op0=ALU.max),
                   reads=["B5"], writes=["rdenA"])
                op("dve", lambda e: e.reciprocal(out=rdenA[:], in_=rdenA[:]), reads=["rdenA"], writes=["rdenA"])
                oAb = oA[sb % 2]
                oreg = "oA%d" % (sb % 2)
                op("dve", lambda e, oAb=oAb: e.tensor_tensor(out=oAb[:].rearrange("p (h d) -> p h d", h=6),
                                                            in0=bk(5, 0, 390).rearrange("p (h d) -> p h d", h=6)[:, :, 0:64],
                                                            in1=rdenA[:].unsqueeze(2).to_broadcast([128, 6, 64]), op=ALU.mult),
                   reads=["B5", "rdenA"], writes=[oreg])
                op("pool", lambda e, oAb=oAb, sb=sb: e.dma_start(out=o_scr[sb * 128:(sb + 1) * 128, 0:384], in_=oAb[:]),
                   reads=[oreg], writes=["oscr%d" % sb], dma_sem=oreg)

        def pass_B():
          with contextlib.ExitStack() as st:
            winB = sbt(st, "winB", [128, 8, 1184], BF16)
            tabN = [sbt(st, "tabN%d" % k, [128, 15, 640], BF16) for k in range(2)]
            cfar = sbt(st, "cfar", [128, 640], F32)
            EXs = sbt(st, "EXs", [32, 16 * 128], BF16)
            ovs = sbt(st, "ovs", [128, NCT, NS], BF16)
            ksT = sbt(st, "ksT", [128, TX], BF16)
            vs = sbt(st, "vs", [128, NBX, 2, 65], BF16)
            kwT = sbt(st, "kwT", [128, RW * 128], BF16)
            vw = sbt(st, "vw", [128, RW, 2, 65], BF16)
            qT = sbt(st, "qT", [128, 5, 128], BF16)
            gsig = sbt(st, "gsig", [128, 30], F32)
            cb = [sbt(st, "cb%d" % i, [128, 640], F32) for i in range(2)]
            scs = sbt(st, "scs", [128, 640], F32)
            PTs = [sbt(st, "PTb%d" % i, [128, 640], BF16) for i in range(3)]
            PT2 = [sbt(st, "PT2b%d" % i, [128, 640], BF16) for i in range(2)]
            smul = [sbt(st, "smul%d" % i, [128, NS], F32) for i in range(2)]
            sadd = [sbt(st, "sadd%d" % i, [128, NS], F32) for i in range(2)]
            NS8 = ((NS + 7) // 8) * 8 + 8
            imp = sbt(st, "imp", [128, NS8], F32)
            impw = sbt(st, "impw", [128, NS8], F32)
            mx = sbt(st, "mx", [128, 16], F32)
            selm = sbt(st, "selm", [128, NSG * 32], BF16)
            selT = sbt(st, "selT", [32, NSG, 128], BF16)
            rden = sbt(st, "rden", [128, 3, 5], F32)
            coef = sbt(st, "coef", [128, 3, 5], F32)
            ob = sbt(st, "ob", [128, 320], F32)
            ob2 = sbt(st, "ob2", [128, 320], F32)
            oB = [sbt(st, "oB%d" % i, [128, 640], BF16) for i in range(2)]
            xb = make_xbufs(st)

            op("pool", lambda e: e.memset(winB[:, :, 1182:1184], 0.0), writes=["wcast"])
            for kc in range(8):
                op("sp", lambda e, kc=kc: e.dma_start(out=stg[:, 0:640], in_=w_in[kc * 128:(kc + 1) * 128, 1152:1792]), writes=["stg"], dma_sem="stg")
                op("dve", lambda e, kc=kc: e.tensor_copy(out=winB[:, kc, 0:640].rearrange("p (g k d) -> p k g d", g=5, k=2),
                                                         in_=stg[:, 0:640].rearrange("p (k g d) -> p k g d", k=2, g=5)), reads=["stg"], writes=["wcast"])
                op("sp", lambda e, kc=kc: e.dma_start(out=stg[:, 0:542], in_=w_in[kc * 128:(kc + 1) * 128, 2048:2590]), writes=["stg"], dma_sem="stg")
                for (so, do) in ((0, 640), (256, 768), (128, 896), (384, 1024)):
                    op("pool", lambda e, kc=kc, so=so, do=do: e.tensor_copy(out=winB[:, kc, do:do + 128], in_=stg[:, so:so + 128]), reads=["stg"], writes=["wcast"])
                op("pool", lambda e, kc=kc: e.tensor_copy(out=winB[:, kc, 1152:1182], in_=stg[:, 512:542]), reads=["stg"], writes=["wcast"])
            for k in range(2):
                op("sp", lambda e, k=k: e.dma_start(out=cfar[:], in_=tabN_d[k, :, 13, :]), writes=["cfar"], dma_sem="cfar")
                for dl in range(15):
                    op("sp", lambda e, k=k, dl=dl: e.dma_start(out=stg[:, 0:640], in_=tabN_d[k, :, dl, :]), writes=["stg"], dma_sem="stg")
                    op("dve" if dl % 2 else "pool", lambda e, k=k, dl=dl: e.tensor_tensor(out=tabN[k][:, dl, :], in0=stg[:, 0:640], in1=cfar[:], op=ALU.subtract),
                       reads=["stg", "cfar"], writes=["wcast"])
            load_cast(EX_d, EXs[:], 2048, stg_view=stg[0:32, 0:2048])
            load_cast(ov_d.rearrange("p c s -> p (c s)"), ovs[:].rearrange("p c s -> p (c s)"), NCT * NS)
            op("pool", lambda e: e.memset(vs[:, :, :, 64:65], 1.0), writes=["vs%d" % s_ for s_ in range(NSB)])
            op("pool", lambda e: e.memset(vw[:, :, :, 64:65], 1.0), writes=["vw%d" % s_ for s_ in range(0, RW, 2)])
            op("pool", lambda e: e.memset(selm[:], 0.0), writes=["selm"])
            op("pool", lambda e: e.memset(imp[:], -1.0e30), writes=["imp"])

            sbufs = Rot("SB", [0, 2])
            for sb in range(0 if stop == "B_init" else NSB):
                if sb == 1 and stop == "B_1":
                    break
                j = 2 * sb
                xt_t, xt_reg, xnT_t, xnT_reg = load_norm_transpose(xb, sb, 2, 0)
                wslot0 = j % RW
                sm_t, sm_reg = smul[sb % 2], "smul%d" % (sb % 2)
                sa_t, sa_reg = sadd[sb % 2], "sadd%d" % (sb % 2)
                op("sp", lambda e, sb=sb, t=sm_t: e.dma_start(out=t[:], in_=selmul_d[sb]), writes=[sm_reg], dma_sem=sm_reg)
                op("sp", lambda e, sb=sb, t=sa_t: e.dma_start(out=t[:], in_=seladd_d[sb]), writes=[sa_reg], dma_sem=sa_reg)
                chk("B_p0")
                proj_fm(lambda kc: winB[:, kc, 640:768], xnT_t, xnT_reg, 256, ksT[:, j * 128: j * 128 + 256], "ksT%d" % sb, "wcast")
                proj_fm(lambda kc: winB[:, kc, 768:896], xnT_t, xnT_reg, 256, kwT[:, wslot0 * 128: wslot0 * 128 + 256], "kw%d" % wslot0, "wcast")
                chk("B_p1")
                for g in range(5):
                    proj_fm(lambda kc, g=g: winB[:, kc, g * 128:(g + 1) * 128], xnT_t, xnT_reg, 128, qT[:, g, :], "qT", "wcast", scale=0.125)
                chk("B_p2")
                for b in range(2):
                    pv, breg = proj_tm(xnT_t, xnT_reg, b, lambda kc: winB[:, kc, 896:1152], 256, "wcast")
                    ve = "act" if b == 0 else "dve"
                    evac(vs[:, j + b, :, 0:64], pv[:, 0:128].rearrange("p (k d) -> p k d", k=2), [breg], ["vs%d" % sb], eng=ve)
                    evac(vw[:, wslot0 + b, :, 0:64], pv[:, 128:256].rearrange("p (k d) -> p k d", k=2), [breg], ["vw%d" % wslot0], eng=ve)
                    if b == 0:
                        chk("B_p3")
                        pg, greg = proj_tm(xnT_t, xnT_reg, 0, lambda kc: winB[:, kc, 1152:1184], 32, "wcast")
                        op("act", lambda e, pg=pg: e.activation(out=gsig[:], in_=pg[:, 0:30], func=AF.Exp, scale=-1.0), reads=[greg], writes=["gsig"])
                        op("dve", lambda e: e.tensor_scalar(out=gsig[:], in0=gsig[:], scalar1=1.0, scalar2=None, op0=ALU.add), reads=["gsig"], writes=["gsig"])
                        op("dve", lambda e: e.reciprocal(out=gsig[:], in_=gsig[:]), reads=["gsig"], writes=["gsig"])

                chk("B_p")
                oBb = oB[sb % 2]
                oreg = "oB%d" % (sb % 2)
                for kvh in range(2):
                    rows = slice(kvh * 64, (kvh + 1) * 64)
                    qrhs = qT[rows, :, :].rearrange("p g q -> p (g q)")
                    gk = gsig[:, kvh * 15:(kvh + 1) * 15].rearrange("p (g r) -> p g r", r=3)

                    def scores(sbk, lhsT_ap, lreads, tab_ap, sregs):
                        for (lo, hi) in ((0, 512), (512, 640)):
                            outap = ps[:, sbk * 512 + lo: sbk * 512 + hi]
                            op("pe", lambda e, outap=outap, lo=lo, hi=hi: e.matmul(outap, lhsT=lhsT_ap, rhs=qrhs[:, lo:hi], start=True, stop=(tab_ap is None)),
                               reads=lreads + ["qT"], writes=sregs)
                            if tab_ap is not None:
                                op("pe", lambda e, outap=outap, lo=lo, hi=hi: e.matmul(outap, lhsT=identb[:], rhs=tab_ap[:, lo:hi], start=False, stop=True),
                                   reads=["identb", "wcast"], writes=sregs)

                    def pv_mm(obank, pt_ap, ptreg, v_ap, vreg, first, last):
                        for h in range(5):
                            op("pe", lambda e, h=h: e.matmul(bk(obank, h * 65, h * 65 + 65), lhsT=pt_ap[:, h * 128:(h + 1) * 128], rhs=v_ap, start=(first and h == 0), stop=last, skip_group_check=True),
                               reads=[ptreg, vreg], writes=["B%d" % obank])

                    def fin_den(obank, bi):
                        den = bk(obank, 0, 325).rearrange("p (h d) -> p h d", h=5)[:, :, 64]
                        op("dve", lambda e: e.tensor_scalar(out=rden[:, bi, :], in0=den, scalar1=1e-30, scalar2=None, op0=ALU.max), reads=["B%d" % obank], writes=["rden"])
                        op("dve", lambda e: e.reciprocal(out=rden[:, bi, :], in_=rden[:, bi, :]), reads=["rden"], writes=["rden"])
                        op("dve", lambda e: e.tensor_tensor(out=coef[:, bi, :], in0=rden[:, bi, :], in1=gk[:, :, bi], op=ALU.mult), reads=["rden", "gsig"], writes=["coef"])

                    cmax = min((16 * sb + 6) // 128, NCT - 1)
                    pendB = [None]
                    for c in range(cmax + 1):
                        e_ = j - 16 * c
                        ei = min(e_ // 2, 14)
                        cbt, cbreg = cb[(c) % 2], "cb%d" % (c % 2)
                        op("sp", lambda e, ei=ei, cbt=cbt: e.dma_start(out=cbt[:], in_=tabC_d[ei, kvh]), writes=[cbreg], dma_sem=cbreg)
                        sbk = 0
                        sregs = ["B0", "B1"]
                        scores(sbk, kcmpT[rows, c * 128:(c + 1) * 128], ["kcmpT"], None, sregs)
                        op("dve", lambda e, cbt=cbt: e.tensor_tensor(out=scs[:], in0=ps[:, 0:640], in1=cbt[:], op=ALU.add), reads=sregs + [cbreg], writes=["scs"])
                        ptb, ptreg = PTs[c % 3], "PTb%d" % (c % 3)
                        op("act", lambda e, ptb=ptb, c=c: e.activation(out=ptb[:], in_=scs[:], func=AF.Exp, bias=cmpvalid[:, c:c + 1]), reads=["scs", "cmpvalid"], writes=[ptreg])
                        def mk_c(ptb=ptb, ptreg=ptreg, c=c):
                            def f():
                                pv_mm(4, ptb, ptreg, vcmp[:, c, kvh, :], "vcmp", c == 0, c == cmax)
                                for h in range(5):
                                    ib = 2 + h // 3
                                    op("pe", lambda e, h=h, ib=ib: e.matmul(bk(ib, (h % 3) * NS, (h % 3) * NS + NS), lhsT=ptb[:, h * 128:(h + 1) * 128],
                                                                            rhs=ovs[:, c, :], start=(c == 0 and h % 3 == 0), stop=(c == cmax), skip_group_check=True),
                                       reads=[ptreg, "wcast"], writes=["B%d" % ib])
                            return f
                        if pendB[0] is not None:
                            pendB[0]()
                        pendB[0] = mk_c()
                    if pendB[0] is not None:
                        pendB[0]()
                        pendB[0] = None
                    chk("B_c")
                    fin_den(4, 0)
                    for h in range(5):
                        ib = 2 + h // 3
                        src = bk(ib, (h % 3) * NS, (h % 3) * NS + NS)
                        if h == 0:
                            op("dve", lambda e, src=src: e.tensor_scalar(out=imp[:, 0:NS], in0=src, scalar1=rden[:, 0, 0:1], scalar2=None, op0=ALU.mult),
                               reads=["B%d" % ib, "rden"], writes=["imp"])
                        else:
                            op("dve", lambda e, src=src, h=h: e.scalar_tensor_tensor(out=imp[:, 0:NS], in0=src, scalar=rden[:, 0, h:h + 1], in1=imp[:, 0:NS], op0=ALU.mult, op1=ALU.add),
                               reads=["B%d" % ib, "rden", "imp"], writes=["imp"])
                    op("dve", lambda e, t=sm_t: e.tensor_tensor(out=imp[:, 0:NS], in0=imp[:, 0:NS], in1=t[:], op=ALU.mult), reads=["imp", sm_reg], writes=["imp"])
                    op("dve", lambda e, t=sa_t: e.tensor_tensor(out=imp[:, 0:NS], in0=imp[:, 0:NS], in1=t[:], op=ALU.add), reads=["imp", sa_reg], writes=["imp"])
                    chk("B_i")
                    op("dve", lambda e: e.max(out=mx[:, 0:8], in_=imp[:]), reads=["imp"], writes=["mx"])
                    op("dve", lambda e: e.match_replace(out=impw[:], in_to_replace=mx[:, 0:8], in_values=imp[:], imm_value=-1.0e9), reads=["mx", "imp"], writes=["impw"])
                    op("dve", lambda e: e.max(out=mx[:, 8:16], in_=impw[:]), reads=["impw"], writes=["mx"])
                    op("dve", lambda e: e.tensor_scalar(out=selm[:, 0:NS], in0=imp[:, 0:NS], scalar1=mx[:, 15:16], scalar2=None, op0=ALU.is_ge), reads=["imp", "mx"], writes=["selm"])
                    for g in range(NSG):
                        op("pe", lambda e, g=g: e.matmul(ps[0:32, 7 * 512 + (g % 4) * 128: 7 * 512 + (g % 4 + 1) * 128], lhsT=selm[:, g * 32:(g + 1) * 32], rhs=identb[:], start=True, stop=True),
                           reads=["selm", "identb"], writes=["B7"])
                        if g % 4 == 3 or g == NSG - 1:
                            g0 = g - g % 4
                            ng = g - g0 + 1
                            op("dve", lambda e, g0=g0, ng=ng: e.tensor_copy(out=selT[:, g0:g0 + ng, :], in_=ps[0:32, 7 * 512: 7 * 512 + ng * 128].rearrange("p (g q) -> p g q", g=ng)),
                               reads=["B7"], writes=["selT"])
                    chk("B_t")
                    op("dve", lambda e: e.tensor_tensor(out=ob[:].rearrange("p (h d) -> p h d", h=5), in0=bk(4, 0, 325).rearrange("p (h d) -> p h d", h=5)[:, :, 0:64],
                                                        in1=coef[:, 0, :].unsqueeze(2).to_broadcast([128, 5, 64]), op=ALU.mult), reads=["B4", "coef"], writes=["ob"])

                    for kb in range(0, j + 1):
                        dl = min(j - kb, 13)
                        sbk, _ = sbufs.next()
                        sregs = ["B%d" % sbk, "B%d" % (sbk + 1)]
                        scores(sbk, ksT[rows, kb * 128:(kb + 1) * 128], ["ksT%d" % (kb // 2)], (tabN[kvh][:, dl, :] if j - kb < 13 else None), sregs)
                        mreg = "B%d" % (6 if kb % 2 else 7)
                        mb = 6 if kb % 2 else 7
                        op("pe", lambda e, kb=kb, mb=mb: e.matmul(bk(mb, 0, 128), lhsT=EXs[:, (kb % 16) * 128:(kb % 16 + 1) * 128], rhs=selT[:, (2 * kb) // 32, :], start=True, stop=True),
                           reads=["selT", "wcast"], writes=[mreg])
                        ptb, ptreg = PTs[kb % 3], "PTb%d" % (kb % 3)
                        op("act", lambda e, sbk=sbk, ptb=ptb, kb=kb: e.activation(out=ptb[:], in_=ps[:, sbk * 512: sbk * 512 + 640], func=AF.Exp, bias=keyvalid[:, kb:kb + 1]),
                           reads=sregs + ["keyvalid"], writes=[ptreg])
                        p2, p2reg = PT2[kb % 2], "PT2b%d" % (kb % 2)
                        op("dve", lambda e, ptb=ptb, p2=p2, mb=mb: e.tensor_tensor(out=p2[:].rearrange("p (h q) -> p h q", h=5), in0=ptb[:].rearrange("p (h q) -> p h q", h=5),
                                                                                   in1=bk(mb, 0, 128).unsqueeze(1).to_broadcast([128, 5, 128]), op=ALU.mult),
                           reads=[ptreg, mreg], writes=[p2reg])
                        def mk_s(p2=p2, p2reg=p2reg, kb=kb):
                            def f():
                                pv_mm(5, p2, p2reg, vs[:, kb, kvh, :], "vs%d" % (kb // 2), kb == 0, kb == j)
                            return f
                        if pendB[0] is not None:
                            pendB[0]()
                        pendB[0] = mk_s()
                    if pendB[0] is not None:
                        pendB[0]()
                        pendB[0] = None
                    chk("B_s")
                    fin_den(5, 1)
                    op("dve", lambda e: e.tensor_tensor(out=ob2[:].rearrange("p (h d) -> p h d", h=5), in0=bk(5, 0, 325).rearrange("p (h d) -> p h d", h=5)[:, :, 0:64],
                                                        in1=coef[:, 1, :].unsqueeze(2).to_broadcast([128, 5, 64]), op=ALU.mult), reads=["B5", "coef"], writes=["ob2"])
                    op("dve", lambda e: e.tensor_tensor(out=ob[:], in0=ob[:], in1=ob2[:], op=ALU.add), reads=["ob", "ob2"], writes=["ob"])

                    kb0 = max(0, j - 4)
                    for kb in range(kb0, j + 1):
                        dl = j - kb
                        ti = 14 if dl == 4 else dl
                        wslot = kb % RW
                        sbk, _ = sbufs.next()
                        sregs = ["B%d" % sbk, "B%d" % (sbk + 1)]
                        scores(sbk, kwT[rows, wslot * 128:(wslot + 1) * 128], ["kw%d" % (wslot - wslot % 2)], tabN[kvh][:, ti, :], sregs)
                        ptb, ptreg = PTs[kb % 3], "PTb%d" % (kb % 3)
                        op("act", lambda e, sbk=sbk, ptb=ptb, kb=kb: e.activation(out=ptb[:], in_=ps[:, sbk * 512: sbk * 512 + 640], func=AF.Exp, bias=keyvalid[:, kb:kb + 1]),
                           reads=sregs + ["keyvalid"], writes=[ptreg])
                        def mk_w(ptb=ptb, ptreg=ptreg, kb=kb, wslot=wslot):
                            def f():
                                pv_mm(4, ptb, ptreg, vw[:, wslot, kvh, :], "vw%d" % (wslot - wslot % 2), kb == kb0, kb == j)
                            return f
                        if pendB[0] is not None:
                            pendB[0]()
                        pendB[0] = mk_w()
                    if pendB[0] is not None:
                        pendB[0]()
                        pendB[0] = None
                    chk("B_w")
                    fin_den(4, 2)
                    op("dve", lambda e: e.tensor_tensor(out=ob2[:].rearrange("p (h d) -> p h d", h=5), in0=bk(4, 0, 325).rearrange("p (h d) -> p h d", h=5)[:, :, 0:64],
                                                        in1=coef[:, 2, :].unsqueeze(2).to_broadcast([128, 5, 64]), op=ALU.mult), reads=["B4", "coef"], writes=["ob2"])
                    op("dve", lambda e, oBb=oBb, kvh=kvh: e.tensor_tensor(out=oBb[:, kvh * 320:(kvh + 1) * 320], in0=ob[:], in1=ob2[:], op=ALU.add),
                       reads=["ob", "ob2"], writes=[oreg])
                op("pool", lambda e, oBb=oBb, sb=sb: e.dma_start(out=o_scr[sb * 128:(sb + 1) * 128, 384:1024], in_=oBb[:]),
                   reads=[oreg], writes=["oscr%d" % sb], dma_sem=oreg)

        def pass_C():
          with contextlib.ExitStack() as st:
            wout = sbt(st, "wout", [128, 8, D], BF16)
            wrs = sbt(st, "wrs", [128, 8, 20], F32)
            brs = sbt(st, "brs", [128, 20], F32)
            hacc = [sbt(st, "hacc%d" % g, [128, D], F32) for g in range(G)]
            xn2T = [sbt(st, "xn2T%d" % g, [128, 8, 128], BF16) for g in range(G)]
            comb = [sbt(st, "comb%d" % g, [128, 16], F32) for g in range(G)]
            xc = [sbt(st, "xc%d" % i, [128, D], F32) for i in range(2)]
            oc = [sbt(st, "oc%d" % i, [128, D], BF16) for i in range(2)]
            oT = sbt(st, "oT", [128, 8, 128], BF16)
            xn2f = sbt(st, "xn2f", [128, D], F32)
            xn2b = sbt(st, "xn2b", [128, D], BF16)
            xn2Tf = sbt(st, "xn2Tf", [128, 8, 128], F32)
            ssq = sbt(st, "ssqc", [128, 2], F32)
            tmpn = sbt(st, "tmpnc", [128, 2], F32)
            junk = sbt(st, "junkc", [128, D], BF16)
            lg = sbt(st, "lg", [128, 20], F32)
            rt = sbt(st, "rt", [128, 64], F32)
            wst = [sbt(st, "wst%d" % i, [128, 4, 512], F32) for i in range(3)]
            wgb = [sbt(st, "wgb%d" % i, [128, 8, DE], BF16) for i in range(2)]
            wub = [sbt(st, "wub%d" % i, [128, 8, DE], BF16) for i in range(2)]
            wdb = [sbt(st, "wdb%d" % i, [128, 4, D], BF16) for i in range(2)]
            sgs = [sbt(st, "sg%d" % i, [128, DE], F32) for i in range(2)]
            hes = [sbt(st, "he%d" % i, [128, DE], BF16) for i in range(2)]
            heTs = [sbt(st, "heT%d" % i, [128, 4, 128], BF16) for i in range(2)]
            pcnt = [0]
            yout = [sbt(st, "yout%d" % i, [128, D], F32) for i in range(1)]

            for kc in range(8):
                load_cast(w_out[kc * 128:(kc + 1) * 128, :], wout[:, kc, :], 1024, eng="dve" if kc % 2 else "pool")
            op("sp", lambda e: e.dma_start(out=wrs[:], in_=wr_d.rearrange("(c p) n -> p c n", p=128)), writes=["wrs"], dma_sem="cst2", group=True)
            op("sp", lambda e: e.dma_start(out=brs[:], in_=br_d.rearrange("a n -> (a n)").partition_broadcast(128)), writes=["brs"], dma_sem="cst2", group=True)

            wst_rot = Rot("wst", wst)
            ngroups = 0 if stop == "C_init" else (NSB + G - 1) // G
            ybuf_i = [0]
            for grp in range(ngroups):
                blks = list(range(grp * G, min(NSB, (grp + 1) * G)))
                for gi, sb in enumerate(blks):
                    xct, xreg = xc[gi % 2], "xc%d" % (gi % 2)
                    oct_, ocreg = oc[gi % 2], "oc%d" % (gi % 2)
                    op("sp", lambda e, sb=sb, t=xct: e.dma_start(out=t[:], in_=xs[sb * 256: sb * 256 + 128, :]), writes=[xreg], dma_sem=xreg)
                    op("sp", lambda e, sb=sb, t=oct_: e.dma_start(out=t[:], in_=o_scr[sb * 128:(sb + 1) * 128, :]), reads=["oscr%d" % sb], writes=[ocreg], dma_sem=ocreg)
                    transpose_block(lambda k, t=oct_: t[:, k * 128:(k + 1) * 128], ocreg, oT[:], "oT", 8, 7, eng="act")
                    for n in range(2):
                        for kc in range(8):
                            op("pe", lambda e, n=n, kc=kc: e.matmul(bk(n), lhsT=oT[:, kc, :], rhs=wout[:, kc, n * 512:(n + 1) * 512], start=(kc == 0), stop=(kc == 7)),
                               reads=["oT", "wcast"], writes=["B%d" % n])
                    hreg = "hacc%d" % gi
                    op("dve", lambda e, gi=gi, t=xct: e.tensor_tensor(out=hacc[gi][:], in0=ps[:, 0:1024], in1=t[:], op=ALU.add), reads=["B0", "B1", xreg], writes=[hreg])
                    rmsnorm_block(hacc[gi][:], hreg, 1, xn2f[:], "xn2f", ssq[:, 0:1], tmpn[:, 0:1], junk[:], "c")
                    op("pool", lambda e: e.tensor_copy(out=xn2b[:], in_=xn2f[:]), reads=["xn2f"], writes=["xn2b"])
                    transpose_block(lambda k: xn2b[:, k * 128:(k + 1) * 128], "xn2b", xn2T[gi][:], "xn2T%d" % gi, 8, 7, eng="dve")
                    for half in range(2):
                        transpose_block(lambda k, half=half: xn2f[:, (half * 4 + k) * 128:(half * 4 + k + 1) * 128], "xn2f", xn2Tf[:, half * 4:(half + 1) * 4, :], "xn2Tf", 4, 6, fp32=True, eng="act")
                    for kc in range(8):
                        op("pe", lambda e, kc=kc: e.matmul(bk(5, 0, 20), lhsT=xn2Tf[:, kc, :], rhs=wrs[:, kc, :], start=(kc == 0), stop=(kc == 7)),
                           reads=["xn2Tf", "wrs"], writes=["B5"])
                    op("dve", lambda e: e.tensor_tensor(out=lg[:], in0=bk(5, 0, 20), in1=brs[:], op=ALU.add), reads=["B5", "brs"], writes=["lg"])
                    R = "rt"
                    def dv(fn, reads=(R, "lg"), writes=(R,)):
                        op("dve", fn, reads=list(reads), writes=list(writes))
                    dv(lambda e: e.tensor_reduce(out=rt[:, 0:1], in_=lg[:, 0:4], axis=mybir.AxisListType.X, op=ALU.max))
                    dv(lambda e: e.tensor_scalar(out=rt[:, 8:12], in0=lg[:, 0:4], scalar1=rt[:, 0:1], scalar2=None, op0=ALU.is_ge))
                    dv(lambda e: e.tensor_scalar(out=rt[:, 1:5], in0=lg[:, 0:4], scalar1=rt[:, 0:1], scalar2=None, op0=ALU.subtract))
                    dv(lambda e: e.memset(rt[:, 5:6], 0.0))
                    op("act", lambda e: e.activation(out=rt[:, 1:5], in_=rt[:, 1:5], func=AF.Exp, accum_out=rt[:, 5:6]), reads=[R], writes=[R])
                    dv(lambda e: e.reciprocal(out=rt[:, 6:7], in_=rt[:, 5:6]))
                    dv(lambda e: e.tensor_scalar(out=rt[:, 12:16], in0=lg[:, 4:8], scalar1=rt[:, 8:9], scalar2=None, op0=ALU.mult))
                    for g in range(1, 4):
                        dv(lambda e, g=g: e.scalar_tensor_tensor(out=rt[:, 12:16], in0=lg[:, 4 + 4 * g: 8 + 4 * g], scalar=rt[:, 8 + g: 9 + g], in1=rt[:, 12:16], op0=ALU.mult, op1=ALU.add))
                    dv(lambda e: e.tensor_reduce(out=rt[:, 16:17], in_=rt[:, 12:16], axis=mybir.AxisListType.X, op=ALU.max))
                    dv(lambda e: e.tensor_scalar(out=rt[:, 20:24], in0=rt[:, 12:16], scalar1=rt[:, 16:17], scalar2=None, op0=ALU.is_ge))
                    dv(lambda e: e.scalar_tensor_tensor(out=rt[:, 28:32], in0=rt[:, 20:24], scalar=-1.0e9, in1=rt[:, 12:16], op0=ALU.mult, op1=ALU.add))
                    dv(lambda e: e.tensor_reduce(out=rt[:, 17:18], in_=rt[:, 28:32], axis=mybir.AxisListType.X, op=ALU.max))
                    dv(lambda e: e.tensor_scalar(out=rt[:, 24:28], in0=rt[:, 28:32], scalar1=rt[:, 17:18], scalar2=None, op0=ALU.is_ge))
                    dv(lambda e: e.tensor_tensor(out=rt[:, 32:33], in0=rt[:, 17:18], in1=rt[:, 16:17], op=ALU.subtract))
                    op("act", lambda e: e.activation(out=rt[:, 33:34], in_=rt[:, 32:33], func=AF.Exp), reads=[R], writes=[R])
                    dv(lambda e: e.tensor_scalar(out=rt[:, 34:35], in0=rt[:, 33:34], scalar1=1.0, scalar2=None, op0=ALU.add))
                    dv(lambda e: e.reciprocal(out=rt[:, 35:36], in_=rt[:, 34:35]))
                    dv(lambda e: e.tensor_tensor(out=rt[:, 36:37], in0=rt[:, 33:34], in1=rt[:, 35:36], op=ALU.mult))
                    dv(lambda e: e.tensor_scalar(out=rt[:, 40:44], in0=rt[:, 20:24], scalar1=rt[:, 35:36], scalar2=None, op0=ALU.mult))
                    dv(lambda e: e.scalar_tensor_tensor(out=rt[:, 40:44], in0=rt[:, 24:28], scalar=rt[:, 36:37], in1=rt[:, 40:44], op0=ALU.mult, op1=ALU.add))
                    dv(lambda e: e.tensor_scalar(out=rt[:, 40:44], in0=rt[:, 40:44], scalar1=rt[:, 6:7], scalar2=None, op0=ALU.mult))
                    for g in range(4):
                        op("dve", lambda e, g=g, gi=gi: e.tensor_scalar(out=comb[gi][:, 4 * g: 4 * g + 4], in0=rt[:, 40:44], scalar1=rt[:, 8 + g: 9 + g], scalar2=None, op0=ALU.mult),
                           reads=[R], writes=["comb%d" % gi])

                pendC = [None]
                for ex in range(NE):
                    wslot = ex % 2
                    for (wd_, dstb, nm) in ((wg_d, wgb, "wg"), (wu_d, wub, "wu")):
                        for hf in range(2):
                            t, treg = wst_rot.next()
                            op("sp", lambda e, wd_=wd_, hf=hf, t=t, ex=ex: e.dma_start(out=t[:], in_=wd_[ex, hf * 512:(hf + 1) * 512, :].rearrange("(c p) n -> p c n", p=128)),
                               writes=[treg], dma_sem=treg)
                            op("pool", lambda e, t=t, dstb=dstb, hf=hf, wslot=wslot: e.tensor_copy(out=dstb[wslot][:, hf * 4:(hf + 1) * 4, :], in_=t[:]),
                               reads=[treg], writes=["%s%d" % (nm, wslot)])
                    for hf in range(2):
                        t, treg = wst_rot.next()
                        op("sp", lambda e, hf=hf, t=t, ex=ex: e.dma_start(out=t[:].rearrange("p (c a) n -> p c (a n)", c=2),
                                                                         in_=wd_d[ex, hf * 256:(hf + 1) * 256, :].rearrange("(c p) n -> p c n", p=128)),
                           writes=[treg], dma_sem=treg)
                        op("pool", lambda e, t=t, hf=hf, wslot=wslot: e.tensor_copy(out=wdb[wslot][:, hf * 2:(hf + 1) * 2, :], in_=t[:].rearrange("p (c a) n -> p c (a n)", c=2)),
                           reads=[treg], writes=["wd%d" % wslot])
                    for gi, sb in enumerate(blks):
                        gb = 0 if gi % 2 == 0 else 2
                        for (n, wb, nm) in ((0, wgb, "wg"), (1, wub, "wu")):
                            for kc in range(8):
                                op("pe", lambda e, n=n, wb=wb, kc=kc, gi=gi, gb=gb: e.matmul(bk(gb + n), lhsT=xn2T[gi][:, kc, :], rhs=wb[wslot][:, kc, :], start=(kc == 0), stop=(kc == 7)),
                                   reads=["xn2T%d" % gi, "%s%d" % (nm, wslot)], writes=["B%d" % (gb + n)])
                        pi = pcnt[0] % 2
                        pcnt[0] += 1
                        sg_t, he_t, heT_t = sgs[pi], hes[pi], heTs[pi]
                        op("act", lambda e, gb=gb, sg_t=sg_t: e.activation(out=sg_t[:], in_=bk(gb), func=AF.Silu), reads=["B%d" % gb], writes=["sg%d" % pi])
                        op("dve", lambda e, gb=gb, gi=gi, ex=ex, sg_t=sg_t, he_t=he_t: e.scalar_tensor_tensor(out=he_t[:], in0=bk(gb + 1), scalar=comb[gi][:, ex:ex + 1], in1=sg_t[:], op0=ALU.mult, op1=ALU.mult),
                           reads=["B%d" % (gb + 1), "comb%d" % gi, "sg%d" % pi], writes=["he%d" % pi])

                        def mk_c(pi=pi, he_t=he_t, heT_t=heT_t, gi=gi, wslot=wslot):
                            def f():
                                transpose_block(lambda k: he_t[:, k * 128:(k + 1) * 128], "he%d" % pi, heT_t[:], "heT%d" % pi, 4, 6, eng="act")
                                for n in range(2):
                                    for dc in range(4):
                                        op("pe", lambda e, n=n, dc=dc: e.matmul(bk(4 + n), lhsT=heT_t[:, dc, :], rhs=wdb[wslot][:, dc, n * 512:(n + 1) * 512], start=(dc == 0), stop=(dc == 3)),
                                           reads=["heT%d" % pi, "wd%d" % wslot], writes=["B%d" % (4 + n)])
                                op("dve", lambda e: e.tensor_tensor(out=hacc[gi][:], in0=hacc[gi][:], in1=ps[:, 4 * 512: 6 * 512], op=ALU.add),
                                   reads=["B4", "B5", "hacc%d" % gi], writes=["hacc%d" % gi])
                            return f
                        if pendC[0] is not None:
                            pendC[0]()
                        pendC[0] = mk_c()
                if pendC[0] is not None:
                    pendC[0]()
                    pendC[0] = None
                for gi, sb in enumerate(blks):
                    yb, yreg = yout[0], "yout0"
                    ybuf_i[0] += 1
                    rmsnorm_block(hacc[gi][:], "hacc%d" % gi, 2, yb[:], yreg, ssq[:, 1:2], tmpn[:, 1:2], junk[:], "f")
                    last_tok = op("pool", lambda e, yb=yb, sb=sb: e.dma_start(out=out_d[sb * 128:(sb + 1) * 128, :], in_=yb[:]), reads=[yreg], writes=["out%d" % sb], dma_sem=yreg)

        pass_A()
        if stop not in ("A_init", "A_1", "A"):
            S.barrier()
            pass_B()
            if stop not in ("B_init", "B_1", "B"):
                S.barrier()
                pass_C()
        finals = [("D", n, v[0]) for n, v in S.dma_sems.items() if n.startswith("yout") or n.startswith("oA") or n.startswith("oB")]
        S.emit(final_wait_tokens=finals)
    return nc


_PROG_CACHE = {}


def _prep_common(NB, inp):
    stt = _struct_tables(NB)
    relb = np.concatenate([np.asarray(inp["rel_bias"], np.float32), np.full((1, 16), NEG, np.float32)], axis=0)
    tN = relb[stt["idxN"]]
    tabN = np.stack([tN[..., 6 + 5 * k: 11 + 5 * k].transpose(0, 1, 3, 2).reshape(128, 15, 640) for k in range(2)], 0)
    tA = relb[stt["idxA"]][..., 0:6]
    tabA = tA.transpose(0, 1, 3, 2).reshape(128, 17, 768)
    tC = relb[stt["idxC"]]
    tabC = np.stack([tC[..., 6 + 5 * k: 11 + 5 * k].transpose(1, 0, 3, 2).reshape(15, 128, 640) for k in range(2)], 1)
    c = dict(
        w_in=np.ascontiguousarray(inp["w_in"][0]), w_out=np.ascontiguousarray(inp["w_out"][0]),
        gvec=np.ascontiguousarray(np.stack([inp["norm_mix"][0], inp["norm_ffn"][0], inp["norm_final"]], 0)),
        tabN=np.ascontiguousarray(tabN), tabA=np.ascontiguousarray(tabA), logm=stt["logm"], tabC=np.ascontiguousarray(tabC),
        ov=stt["ov"], EX=stt["EX"].reshape(32, 2048), E16=stt["E16"],
        posT=np.ascontiguousarray(np.stack([inp["cmp_pos_k"][0].T, inp["cmp_pos_v"][0].T], 0)),
        w1=np.ascontiguousarray(np.stack([inp["cmp_k_w1"][0], inp["cmp_v_w1"][0]], 0)),
        w2=np.ascontiguousarray(np.stack([inp["cmp_k_w2"][0], inp["cmp_v_w2"][0]], 0)),
        wr=np.ascontiguousarray(np.concatenate([inp["w_router_group"][0], inp["w_router_expert"][0].reshape(D, 16)], axis=1)),
        br=np.ascontiguousarray(np.concatenate([inp["b_router_group"][0], inp["b_router_expert"][0].reshape(16)])[None, :]),
        wg=np.ascontiguousarray(inp["w_gate"][0]), wu=np.ascontiguousarray(inp["w_up"][0]), wd=np.ascontiguousarray(inp["w_down"][0]),
    )
    return {k: np.asarray(v, np.float32) for k, v in c.items()}


def run(inp, debug=False, G=9, stop=None):
    x = np.asarray(inp["x"], np.float32)
    B, T, _ = x.shape
    NB = T // 128
    NBX = NB + 2
    NSB = NBX // 2
    key = (NB, debug, G)
    if key not in _PROG_CACHE:
        _PROG_CACHE[key] = build_program(NB, G=G, debug=debug, stop=stop)
    nc = _PROG_CACHE[key]
    common = _prep_common(NB, inp)
    ctabs = [_core_tables(NB, p) for p in range(2)]
    in_maps = []
    for c in range(2 * B):
        b, p = c // 2, c % 2
        xs = np.zeros((NBX * 128, D), np.float32)
        xs[p * 128: p * 128 + T] = x[b]
        m = dict(common)
        m["xs"] = xs
        m.update(ctabs[p])
        in_maps.append(m)
    res = run_bass_kernel_spmd(nc, in_maps, core_ids=list(range(2 * B)))
    out = np.zeros((B, T, D), np.float32)
    dbg = np.zeros((B, T, D), np.float32) if debug else None
    for c in range(2 * B):
        b, p = c // 2, c % 2
        o = np.asarray(res.results[c]["out"]).reshape(NSB, 128, D)
        for i in range(NSB):
            r = 2 * i - p
            if 0 <= r < NB:
                out[b, r * 128:(r + 1) * 128] = o[i]
                if debug:
                    dbg[b, r * 128:(r + 1) * 128] = np.asarray(res.results[c]["o_scr"]).astype(np.float32).reshape(NSB, 128, D)[i]
    if debug:
        return out, dbg
    return out


def kernel(**inputs):
    return run(inputs)
```
